# Optimizing a Trainium2 kernel written in Bass

```python
import math
import jax, jax.numpy as jnp
from jax import lax
import numpy as np

D_MODEL = 1024
BATCH = 16
SEQ = 2048
DEPTH = 2

N_META = 16
N_A_LAYERS = DEPTH // 2
N_B_LAYERS = DEPTH - N_A_LAYERS
CONV_WIDTH = 3
DIFF_HEAD_DIM = 64
N_DIFF_HEADS = D_MODEL // (2 * DIFF_HEAD_DIM)
DIFF_WIDTH = N_DIFF_HEADS * 2 * DIFF_HEAD_DIM
Q_BLOCK = 128
N_GROUPS = 4
EXPERTS_PER_GROUP = 4
N_EXPERTS = N_GROUPS * EXPERTS_PER_GROUP
TOP_K_IN_GROUP = 2
D_EXPERT = D_MODEL // 2
RMS_EPS = 1e-6
SUBLN_EPS = 1e-5

kernel_name = 'yoco_shortconv_diffattn_hmoe'


def rmsnorm(x, g, eps=RMS_EPS):
    xf = x.astype(jnp.float32)
    y = xf * lax.rsqrt(jnp.mean(xf * xf, axis=-1, keepdims=True) + eps)
    return (y * g.astype(jnp.float32)).astype(x.dtype)


def short_conv_mixer(xn, w_in, conv_w, w_out):
    bcu = xn @ w_in
    b, c, u = jnp.split(bcu, 3, axis=-1)
    z = c * u
    conv = lax.conv_general_dilated(
        z, conv_w[:, None, :], window_strides=(1,), padding=[(CONV_WIDTH - 1, 0)],
        dimension_numbers=('NWC', 'WIO', 'NWC'), feature_group_count=D_MODEL)
    return (b * conv) @ w_out


def lambda_init_for(layer_idx):
    return 0.8 - 0.6 * math.exp(-0.3 * layer_idx)


def diff_attention(xn, k, v, w_q, lam, subln_g, w_o, lambda_init):
    bsz, seq_len, _ = xn.shape
    lp = k.shape[1]
    n_blocks = lp // Q_BLOCK
    q = (xn @ w_q).reshape(bsz, seq_len, N_DIFF_HEADS, 2, DIFF_HEAD_DIM)
    q = jnp.pad(q, ((0, 0), (0, lp - seq_len), (0, 0), (0, 0), (0, 0)))
    q_blocks = q.reshape(bsz, n_blocks, Q_BLOCK, N_DIFF_HEADS, 2, DIFF_HEAD_DIM).transpose(1, 0, 2, 3, 4, 5)
    lam_f = lam.astype(jnp.float32)
    lambda_full = jnp.exp(jnp.sum(lam_f[0] * lam_f[1])) - jnp.exp(jnp.sum(lam_f[2] * lam_f[3])) + lambda_init
    key_pos = jnp.arange(lp)
    scale = DIFF_HEAD_DIM ** -0.5

    def one_block(args):
        qb, start = args
        s = jnp.einsum('bqhcd,bkhcd->bhcqk', qb, k).astype(jnp.float32) * scale
        q_pos = start + jnp.arange(Q_BLOCK)
        mask = key_pos[None, :] <= q_pos[:, None]
        s = jnp.where(mask, s, -jnp.inf)
        p = jax.nn.softmax(s, axis=-1)
        a = p[:, :, 0] - lambda_full * p[:, :, 1]
        return jnp.einsum('bhqk,bkhe->bqhe', a.astype(v.dtype), v)

    starts = jnp.arange(n_blocks) * Q_BLOCK
    o = lax.map(one_block, (q_blocks, starts))
    o = o.transpose(1, 0, 2, 3, 4).reshape(bsz, lp, N_DIFF_HEADS, 2 * DIFF_HEAD_DIM)[:, :seq_len]
    o = rmsnorm(o, subln_g, SUBLN_EPS) * (1.0 - lambda_init)
    return o.reshape(bsz, seq_len, DIFF_WIDTH) @ w_o


def hier_moe(xn, w_rg, b_rg, w_re, b_re, w_gate, w_up, w_down):
    bsz, seq_len, d = xn.shape
    t = xn.reshape(-1, d)
    lg = (t @ w_rg).astype(jnp.float32) + b_rg.astype(jnp.float32)
    pg = jax.nn.softmax(lg, axis=-1)
    g_idx = jnp.argmax(lg, axis=-1)
    p_sel = jnp.take_along_axis(pg, g_idx[:, None], axis=-1)[:, 0]
    le = ((t @ w_re).astype(jnp.float32) + b_re.astype(jnp.float32)).reshape(-1, N_GROUPS, EXPERTS_PER_GROUP)
    le_sel = jnp.take_along_axis(le, g_idx[:, None, None], axis=1)[:, 0]
    top_v, top_i = lax.top_k(le_sel, TOP_K_IN_GROUP)
    w_k = jax.nn.softmax(top_v, axis=-1) * p_sel[:, None]
    within = jnp.sum(jax.nn.one_hot(top_i, EXPERTS_PER_GROUP, dtype=jnp.float32) * w_k[..., None], axis=1)
    combine = (jax.nn.one_hot(g_idx, N_GROUPS, dtype=jnp.float32)[:, :, None] * within[:, None, :])
    combine = combine.reshape(-1, N_EXPERTS).astype(t.dtype)
    y = jnp.zeros_like(t)
    for e in range(N_EXPERTS):
        h = jax.nn.silu(t @ w_gate[e]) * (t @ w_up[e])
        y = y + (h * combine[:, e:e + 1]) @ w_down[e]
    return y.reshape(bsz, seq_len, d)


def setup_inputs(seed: int = 0) -> dict:
    key = jax.random.key(seed)
    ks = jax.random.split(key, 24)
    f32 = jnp.float32
    nrm = lambda k, shape, s: jax.random.normal(k, shape, f32) * s
    gain = lambda k, shape: 1.0 + 0.02 * jax.random.normal(k, shape, f32)
    D = D_MODEL
    return {
        'x': nrm(ks[0], (BATCH, SEQ, D), 1.0),
        'meta_tokens': nrm(ks[1], (N_META, D), 1.0),
        'a_norm': gain(ks[2], (N_A_LAYERS, D)),
        'a_w_in': nrm(ks[3], (N_A_LAYERS, D, 3 * D), D ** -0.5),
        'a_conv': nrm(ks[4], (N_A_LAYERS, CONV_WIDTH, D), CONV_WIDTH ** -0.5),
        'a_w_out': nrm(ks[5], (N_A_LAYERS, D, D), D ** -0.5),
        'kv_norm': gain(ks[6], (D,)),
        'w_kv': nrm(ks[7], (D, 2 * DIFF_WIDTH), D ** -0.5),
        'b_norm': gain(ks[8], (N_B_LAYERS, D)),
        'b_w_q': nrm(ks[9], (N_B_LAYERS, D, DIFF_WIDTH), D ** -0.5),
        'b_lambda': nrm(ks[10], (N_B_LAYERS, 4, DIFF_HEAD_DIM), 0.1),
        'b_subln': gain(ks[11], (N_B_LAYERS, 2 * DIFF_HEAD_DIM)),
        'b_w_o': nrm(ks[12], (N_B_LAYERS, DIFF_WIDTH, D), DIFF_WIDTH ** -0.5),
        'ffn_norm': gain(ks[13], (DEPTH, D)),
        'router_group_w': nrm(ks[14], (DEPTH, D, N_GROUPS), D ** -0.5),
        'router_group_b': nrm(ks[15], (DEPTH, N_GROUPS), 0.01),
        'router_expert_w': nrm(ks[16], (DEPTH, D, N_EXPERTS), D ** -0.5),
        'router_expert_b': nrm(ks[17], (DEPTH, N_EXPERTS), 0.01),
        'expert_w_gate': nrm(ks[18], (DEPTH, N_EXPERTS, D, D_EXPERT), D ** -0.5),
        'expert_w_up': nrm(ks[19], (DEPTH, N_EXPERTS, D, D_EXPERT), D ** -0.5),
        'expert_w_down': nrm(ks[20], (DEPTH, N_EXPERTS, D_EXPERT, D), D_EXPERT ** -0.5),
        'final_norm': gain(ks[21], (D,)),
    }


def reference(x, meta_tokens, a_norm, a_w_in, a_conv, a_w_out, kv_norm, w_kv, b_norm, b_w_q, b_lambda,
              b_subln, b_w_o, ffn_norm, router_group_w, router_group_b, router_expert_w, router_expert_b,
              expert_w_gate, expert_w_up, expert_w_down, final_norm):
    bsz = x.shape[0]
    meta = jnp.broadcast_to(meta_tokens[None].astype(x.dtype), (bsz, N_META, D_MODEL))
    h = jnp.concatenate([meta, x], axis=1)
    seq_len = h.shape[1]
    lp = ((seq_len + Q_BLOCK - 1) // Q_BLOCK) * Q_BLOCK
    k_shared = None
    v_shared = None
    for layer in range(DEPTH):
        if layer < N_A_LAYERS:
            i = layer
            h = h + short_conv_mixer(rmsnorm(h, a_norm[i]), a_w_in[i], a_conv[i], a_w_out[i])
        else:
            j = layer - N_A_LAYERS
            if j == 0:
                kv = rmsnorm(h, kv_norm) @ w_kv
                k_part, v_part = jnp.split(kv, 2, axis=-1)
                k_shared = k_part.reshape(bsz, seq_len, N_DIFF_HEADS, 2, DIFF_HEAD_DIM)
                v_shared = v_part.reshape(bsz, seq_len, N_DIFF_HEADS, 2 * DIFF_HEAD_DIM)
                k_shared = jnp.pad(k_shared, ((0, 0), (0, lp - seq_len), (0, 0), (0, 0), (0, 0)))
                v_shared = jnp.pad(v_shared, ((0, 0), (0, lp - seq_len), (0, 0), (0, 0)))
            h = h + diff_attention(rmsnorm(h, b_norm[j]), k_shared, v_shared, b_w_q[j], b_lambda[j],
                                   b_subln[j], b_w_o[j], lambda_init_for(layer))
        h = h + hier_moe(rmsnorm(h, ffn_norm[layer]), router_group_w[layer], router_group_b[layer],
                         router_expert_w[layer], router_expert_b[layer], expert_w_gate[layer],
                         expert_w_up[layer], expert_w_down[layer])
    return rmsnorm(h, final_norm)[:, N_META:]
```

```python
import math
from contextlib import ExitStack

import numpy as np
import concourse.bass as bass
import concourse.mybir as mybir
from concourse.bass_utils import run_bass_kernel_spmd

F32 = mybir.dt.float32
BF16 = mybir.dt.bfloat16
ALU = mybir.AluOpType
AF = mybir.ActivationFunctionType
AX = mybir.AxisListType

D = 1024
KC = 8
SEQ = 2048
NMETA = 16
L = SEQ + NMETA
NCHUNK = 6
N = L // NCHUNK
NSEQ = 2
NEXP = 16
RMS_EPS = 1e-6
SUBLN_EPS = 1e-5
LAMBDA_INIT = 0.8 - 0.6 * math.exp(-0.3 * 1)
BIG = 1.0e30
NRING = 12
NSTG = 2
UNIT = 2048


class Buf:
    __slots__ = ("w", "r", "name")

    def __init__(self, name="", init=None):
        self.w = {}
        self.r = dict(init) if init else {}
        self.name = name


def _merge(dst, src):
    for s, v in src.items():
        if dst.get(s, 0) < v:
            dst[s] = v


class Sched:
    ENGS = ("pe", "act", "dve", "pool", "sp")

    def __init__(self, nc, stack):
        self.nc = nc
        self.stack = stack
        self.nsem = 0
        self.e = {}
        self.dma_slots = []
        self.old = {}
        for n in self.ENGS:
            self.e[n] = dict(ops=[], sem=None, cnt=0, seen={})
        self.epoch()

    def new_sem(self, name):
        s = self.stack.enter_context(self.nc.semaphore(f"{name}{self.nsem}"))
        self.nsem += 1
        return s

    def dma_slot(self, name):
        sl = [self.new_sem(name), 0]
        self.dma_slots.append(sl)
        return sl

    def epoch(self):
        for n in ("pe", "act", "dve", "pool"):
            if self.e[n]["sem"] is not None and self.e[n]["cnt"] > 0:
                self.old[self.e[n]["sem"]] = self.e[n]["cnt"]
            self.e[n]["sem"] = self.new_sem("e" + n)
            self.e[n]["cnt"] = 0

    def barrier(self):
        t = dict(self.old)
        for n in ("pe", "act", "dve", "pool"):
            if self.e[n]["cnt"] > 0:
                t[self.e[n]["sem"]] = self.e[n]["cnt"]
        for sl in self.dma_slots:
            if sl[1] > 0:
                t[sl[0]] = sl[1]
        return t

    def _waits(self, eng, reads, writes, extra):
        waits = {}
        for b in reads:
            _merge(waits, b.w)
        for b in writes:
            _merge(waits, b.w)
            _merge(waits, b.r)
        for t in extra:
            if t is not None:
                _merge(waits, {t[0]: t[1]})
        e = self.e[eng]
        need = []
        for s, v in waits.items():
            if eng == "pe" and s is e["sem"]:
                continue
            if e["seen"].get(s, 0) < v:
                e["seen"][s] = v
                need.append((s, v))
        return need

    def op(self, eng, fn, reads=(), writes=(), extra=()):
        e = self.e[eng]
        need = self._waits(eng, reads, writes, extra)
        e["cnt"] += 1
        tok = (e["sem"], e["cnt"])
        e["ops"].append((need, fn, tok, 1))
        for b in reads:
            _merge(b.r, {tok[0]: tok[1]})
        for b in writes:
            b.w = {tok[0]: tok[1]}
            b.r = {}
        return tok

    def dma(self, queue, fn, semslot, reads=(), writes=(), extra=()):
        e = self.e[queue]
        need = self._waits(queue, reads, writes, extra)
        if semslot[1] > 0 and e["seen"].get(semslot[0], 0) < semslot[1]:
            e["seen"][semslot[0]] = semslot[1]
            need.append((semslot[0], semslot[1]))
        semslot[1] += 16
        tok = (semslot[0], semslot[1])
        e["ops"].append((need, fn, tok, 16))
        for b in reads:
            _merge(b.r, {tok[0]: tok[1]})
        for b in writes:
            b.w = {tok[0]: tok[1]}
            b.r = {}
        return tok

    def wait_only(self, eng, toks):
        need = self._waits(eng, (), (), toks)
        self.e[eng]["ops"].append((need, None, None, 0))

    def replay(self):
        nc = self.nc
        sched = self

        def run(name, eng):
            for need, fn, tok, incv in sched.e[name]["ops"]:
                for s, v in need:
                    eng.wait_ge(s, v)
                if fn is None:
                    continue
                ins = fn(eng)
                ins.then_inc(tok[0], incv)

        with nc.Block() as block:
            @block.tensor
            def _(eng):
                run("pe", eng)

            @block.scalar
            def _(eng):
                run("act", eng)

            @block.vector
            def _(eng):
                run("dve", eng)

            @block.gpsimd
            def _(eng):
                run("pool", eng)

            @block.sync
            def _(eng):
                run("sp", eng)


INPUT_SPECS = [
    ("x", [NSEQ, SEQ, D]), ("meta_tokens", [NMETA, D]), ("a_norm", [1, D]), ("a_w_in", [1, D, 3 * D]),
    ("a_conv", [1, 3, D]), ("a_w_out", [1, D, D]), ("kv_norm", [D]), ("w_kv", [D, 2 * D]),
    ("b_norm", [1, D]), ("b_w_q", [1, D, D]), ("b_lambda", [1, 4, 64]), ("b_subln", [1, 128]),
    ("b_w_o", [1, D, D]), ("ffn_norm", [2, D]), ("router_group_w", [2, D, 4]), ("router_group_b", [2, 4]),
    ("router_expert_w", [2, D, 16]), ("router_expert_b", [2, 16]), ("expert_w_gate", [2, 16, D, 512]),
    ("expert_w_up", [2, 16, D, 512]), ("expert_w_down", [2, 16, 512, D]), ("final_norm", [D]),
]

G_A, G_F0, G_F1, G_KV, G_B, G_FIN = range(6)


def build_nc(nseq=NSEQ, stop_after=None, dbg=False):
    nc = bass.Bass("TRN2", target_bir_lowering=False)
    dr = {}
    for name, shape in INPUT_SPECS:
        dr[name] = nc.dram_tensor(name, shape, F32, kind="ExternalInput").ap()
    out = nc.dram_tensor("out", [NSEQ, SEQ, D], F32, kind="ExternalOutput").ap()
    dbg_out = None
    if dbg:
        dbg_out = nc.dram_tensor("dbg", [128, KC, L], F32, kind="ExternalOutput").ap()

    with ExitStack() as st:
        S = Sched(nc, st)

        def sb(name, shape, dt):
            return st.enter_context(nc.sbuf_tensor(name, shape, dt))

        H = sb("H", [128, KC, L], F32)
        XN = sb("XN", [128, KC, L], BF16)
        RING = sb("RING", [128, NRING, UNIT], BF16)
        STG = sb("STG", [128, NSTG, UNIT], F32)
        ident_f = sb("ident_f", [128, 128], F32)
        ident_b = sb("ident_b", [128, 128], BF16)
        mask01 = sb("mask01", [128, 128], BF16)
        maskneg = sb("maskneg", [128, 128], BF16)
        ones_f = sb("ones_f", [128, 128], F32)
        ones_b = sb("ones_b", [128, 128], BF16)
        SEL = sb("SEL", [128, 16, 128], BF16)
        G = sb("G", [128, 6, KC], F32)
        CW = sb("CW", [128, 3, KC], F32)
        WR32 = sb("WR32", [128, 2, KC, 20], F32)
        WRS = sb("WRS", [128, 2, KC, 20], F32)
        RB = sb("RB", [128, 2, 20], F32)
        GS = sb("GS", [128, 128], F32)
        GSC = sb("GSC", [128, 1], F32)
        NEGLAM = sb("NEGLAM", [128, 1], F32)
        LTMP = sb("LTMP", [128, 8], F32)
        SQ = [sb(f"SQ{i}", [128, N], F32) for i in range(2)]
        RS = [sb(f"RS{i}", [128, N], F32) for i in range(2)]
        SQB = [SQ[i][:, 0:N // 2].bitcast(BF16) for i in range(2)]
        SCR_BYTES = (nc.sbuf_bytes_remaining // 64) * 64 - 256
        SCR = sb("SCR", [128, SCR_BYTES // 4], F32)

        psum = [st.enter_context(nc.psum_tensor(f"ps{i}", [128, 512], F32)) for i in range(8)]
        PB = [Buf(f"ps{i}") for i in range(8)]

        class Scratch:
            def __init__(self):
                self.off = 0

            def reset(self):
                self.off = 0

            def f32(self, shape):
                n = int(np.prod(shape[1:]))
                a = self.off
                self.off += (n + 7) // 8 * 8
                assert self.off * 4 <= SCR_BYTES, (self.off * 4, SCR_BYTES)
                v = SCR[0:shape[0], a:a + n]
                return _reshape(v, shape)

            def bf16(self, shape):
                n = int(np.prod(shape[1:]))
                nw = (n + 1) // 2
                a = self.off
                self.off += (nw + 7) // 8 * 8
                assert self.off * 4 <= SCR_BYTES, (self.off * 4, SCR_BYTES)
                v = SCR[0:shape[0], a:a + nw].bitcast(BF16)[:, 0:n]
                return _reshape(v, shape)

        def _reshape(v, shape):
            if len(shape) == 2:
                return v
            if len(shape) == 3:
                return v.rearrange("p (a b) -> p a b", a=shape[1])
            if len(shape) == 4:
                return v.rearrange("p (a b c) -> p a b c", a=shape[1], b=shape[2])
            raise ValueError

        scr = Scratch()

        def bc(ap2d, mid):
            return ap2d.unsqueeze(1).broadcast_to([ap2d.shape[0], mid, ap2d.shape[1]])

        def bcl(ap2d, last):
            return ap2d.unsqueeze(2).broadcast_to([ap2d.shape[0], ap2d.shape[1], last])

        bH = [Buf(f"H{c}") for c in range(NCHUNK)]
        bXN = [Buf(f"XN{c}") for c in range(NCHUNK)]
        bRING = [Buf(f"ring{i}") for i in range(NRING)]
        bSTG = [Buf(f"stg{i}") for i in range(NSTG)]
        bC = Buf("consts")
        bSQ = [Buf("sq0"), Buf("sq1")]
        bRS = [Buf("rs0"), Buf("rs1")]
        bLT = Buf("ltmp")
        stg_sem = [S.dma_slot("dstg") for _ in range(NSTG)]
        c_sem = S.dma_slot("dconst")
        in_sem = [S.dma_slot("din") for _ in range(4)]
        out_sem = [S.dma_slot("dout") for _ in range(6)]
        dbg_sem = S.dma_slot("ddbg")

        def chunks_of(t0, t1):
            return list(range(t0 // N, (t1 - 1) // N + 1))

        def Hb(t0, t1):
            return [bH[c] for c in chunks_of(t0, t1)]

        def XNb(t0, t1):
            return [bXN[c] for c in chunks_of(t0, t1)]

        def consts():
            S.op("pool", lambda e: e.memset(ident_f[:], 0.0), writes=[bC])
            S.op("pool", lambda e: e.affine_select(out=ident_f[:], in_=ident_f[:], pattern=[[-1, 128]],
                                                   compare_op=ALU.not_equal, fill=1.0, base=0,
                                                   channel_multiplier=1), writes=[bC])
            S.op("pool", lambda e: e.tensor_copy(out=ident_b[:], in_=ident_f[:]), reads=[bC], writes=[bC])
            S.op("pool", lambda e: e.memset(ones_f[:], 1.0), writes=[bC])
            S.op("pool", lambda e: e.memset(ones_b[:], 1.0), writes=[bC])
            tmp = SQ[0][:, 0:128]
            S.op("pool", lambda e: e.memset(tmp, 1.0), writes=[bSQ[0]])
            S.op("pool", lambda e: e.affine_select(out=tmp, in_=tmp, pattern=[[1, 128]], compare_op=ALU.is_ge,
                                                   fill=0.0, base=0, channel_multiplier=-1), writes=[bSQ[0]])
            S.op("pool", lambda e: e.tensor_copy(out=mask01[:], in_=tmp), reads=[bSQ[0]], writes=[bC])
            S.op("pool", lambda e: e.tensor_scalar(out=tmp, in0=tmp, scalar1=-1.0, scalar2=30000.0, op0=ALU.add,
                                                   op1=ALU.mult), reads=[bSQ[0]], writes=[bSQ[0]])
            S.op("pool", lambda e: e.tensor_copy(out=maskneg[:], in_=tmp), reads=[bSQ[0]], writes=[bC])
            scr.reset()
            bt = Buf("selt", S.barrier())
            self_f = scr.f32([128, 16, 128])
            S.op("pool", lambda e: e.memset(self_f, 0.0), writes=[bt])
            S.op("pool", lambda e: e.affine_select(out=self_f, in_=self_f, pattern=[[-1, 16], [0, 128]],
                                                   compare_op=ALU.not_equal, fill=1.0, base=0,
                                                   channel_multiplier=1), writes=[bt])
            S.op("pool", lambda e: e.tensor_copy(out=SEL[:], in_=self_f), reads=[bt], writes=[bC])
            gsrc = [dr["a_norm"][0], dr["ffn_norm"][0], dr["ffn_norm"][1], dr["kv_norm"], dr["b_norm"][0],
                    dr["final_norm"]]
            for i, g in enumerate(gsrc):
                S.dma("sp", lambda e, i=i, g=g: e.dma_start(out=G[:, i, :], in_=g.rearrange("(k p) -> p k", p=128),
                                                            allow_slow_non_contiguous=True), c_sem, writes=[bC])
            S.dma("sp", lambda e: e.dma_start(out=CW[:], in_=dr["a_conv"][0].rearrange("j (k p) -> p j k", p=128),
                                              allow_slow_non_contiguous=True), c_sem, writes=[bC])
            for l in range(2):
                S.dma("sp", lambda e, l=l: e.dma_start(out=WR32[:, l, :, 0:4],
                                                       in_=dr["router_group_w"][l].rearrange("(k p) n -> p k n", p=128)),
                      c_sem, writes=[bC])
                S.dma("sp", lambda e, l=l: e.dma_start(out=WR32[:, l, :, 4:20],
                                                       in_=dr["router_expert_w"][l].rearrange("(k p) n -> p k n", p=128)),
                      c_sem, writes=[bC])
                S.dma("sp", lambda e, l=l: e.dma_start(out=RB[:, l, 0:4],
                                                       in_=dr["router_group_b"][l:l + 1, :].broadcast_to([128, 4])),
                      c_sem, writes=[bC])
                S.dma("sp", lambda e, l=l: e.dma_start(out=RB[:, l, 4:20],
                                                       in_=dr["router_expert_b"][l:l + 1, :].broadcast_to([128, 16])),
                      c_sem, writes=[bC])
            S.dma("sp", lambda e: e.dma_start(out=GS[:], in_=dr["b_subln"][0:1, :].broadcast_to([128, 128])),
                  c_sem, writes=[bC])
            S.dma("sp", lambda e: e.dma_start(out=GSC[:], in_=dr["b_subln"][0].rearrange("(p o) -> p o", o=1)),
                  c_sem, writes=[bC])
            lam = scr.f32([128, 4, 64])
            bl = Buf("lam", S.barrier())
            S.dma("sp", lambda e: e.dma_start(out=lam, in_=dr["b_lambda"][0:1, :, :].broadcast_to([128, 4, 64])),
                  c_sem, writes=[bl])
            for l in range(2):
                S.op("pool", lambda e, l=l: e.tensor_tensor(out=WRS[:, l, :, :], in0=WR32[:, l, :, :],
                                                            in1=bcl(G[:, G_F0 + l, :], 20), op=ALU.mult),
                     reads=[bC], writes=[bC])
            S.op("pool", lambda e: e.tensor_scalar(out=GS[:], in0=GS[:], scalar1=1.0 - LAMBDA_INIT, scalar2=None,
                                                   op0=ALU.mult), reads=[bC], writes=[bC])
            S.op("pool", lambda e: e.tensor_scalar(out=GSC[:], in0=GSC[:], scalar1=1.0 - LAMBDA_INIT, scalar2=None,
                                                   op0=ALU.mult), reads=[bC], writes=[bC])
            p01 = scr.f32([128, 64])
            p23 = scr.f32([128, 64])
            S.op("dve", lambda e: e.tensor_tensor(out=p01, in0=lam[:, 0, :], in1=lam[:, 1, :], op=ALU.mult),
                 reads=[bl], writes=[bt])
            S.op("dve", lambda e: e.reduce_sum(out=LTMP[:, 0:1], in_=p01, axis=AX.X), reads=[bt], writes=[bLT])
            S.op("dve", lambda e: e.tensor_tensor(out=p23, in0=lam[:, 2, :], in1=lam[:, 3, :], op=ALU.mult),
                 reads=[bl], writes=[bt])
            S.op("dve", lambda e: e.reduce_sum(out=LTMP[:, 1:2], in_=p23, axis=AX.X), reads=[bt], writes=[bLT])
            S.op("act", lambda e: e.activation(out=LTMP[:, 2:4], in_=LTMP[:, 0:2], func=AF.Exp), reads=[bLT],
                 writes=[bLT])
            S.op("dve", lambda e: e.tensor_tensor(out=LTMP[:, 4:5], in0=LTMP[:, 3:4], in1=LTMP[:, 2:3],
                                                  op=ALU.subtract), reads=[bLT], writes=[bLT])
            S.op("dve", lambda e: e.tensor_scalar(out=NEGLAM[:], in0=LTMP[:, 4:5], scalar1=-LAMBDA_INIT, scalar2=None,
                                                  op0=ALU.add), reads=[bLT], writes=[bC])

        units = []
        state = dict(dma_next=0, cast_next=0)

        def unitA(w2d, col0, gain):
            units.append((w2d[:, col0:col0 + 256].rearrange("(k p) n -> p k n", p=128), gain, 8, 256))
            return len(units) - 1

        def unitB(w2d, row0, col0):
            units.append((w2d[row0:row0 + 512, col0:col0 + 512].rearrange("(k p) n -> p k n", p=128), None, 4, 512))
            return len(units) - 1

        def unitC(w2d, row0):
            units.append((w2d[row0:row0 + 256, :].rearrange("(k p) n -> p k n", p=128), None, 2, 1024))
            return len(units) - 1

        def ring_view(u):
            _, _, nk, ncol = units[u]
            return RING[:, u % NRING, :].rearrange("p (k n) -> p k n", k=nk)

        def rb(u):
            return bRING[u % NRING]

        def rec_dma(u):
            src, gain, nk, ncol = units[u]
            s = u % NSTG
            dst = STG[:, s, :].rearrange("p (k n) -> p k n", k=nk)
            S.dma("sp", lambda e: e.dma_start(out=dst, in_=src), stg_sem[s], writes=[bSTG[s]])

        def rec_cast(u):
            src, gain, nk, ncol = units[u]
            s = u % NSTG
            stv = STG[:, s, :].rearrange("p (k n) -> p k n", k=nk)
            dst = ring_view(u)
            if gain is None:
                S.op("pool", lambda e: e.tensor_copy(out=dst, in_=stv), reads=[bSTG[s]], writes=[rb(u)])
            else:
                S.op("pool", lambda e: e.tensor_tensor(out=dst, in0=stv, in1=bcl(G[:, gain, :], ncol), op=ALU.mult),
                     reads=[bSTG[s], bC], writes=[rb(u)])

        def ensure_cast(u):
            u = min(u, len(units) - 1)
            while state["cast_next"] <= u:
                j = state["cast_next"]
                while state["dma_next"] <= min(j + NSTG - 1, len(units) - 1):
                    rec_dma(state["dma_next"])
                    state["dma_next"] += 1
                rec_cast(j)
                state["cast_next"] += 1
                while state["dma_next"] <= min(j + NSTG, len(units) - 1):
                    rec_dma(state["dma_next"])
                    state["dma_next"] += 1

        def mm_group(out_ap, pairs, reads, writes):
            def fn(e):
                n = len(pairs)
                ins = None
                for i, (l, r) in enumerate(pairs):
                    ins = e.matmul(out_ap, lhsT=l, rhs=r, start=(i == 0), stop=(i == n - 1))
                return ins
            return S.op("pe", fn, reads=reads, writes=writes)

        def norm_stats(c, ri, rsx=None):
            c0 = c * N
            RS_, bRS_ = (RS[ri], bRS[ri]) if rsx is None else rsx
            for k in range(KC):
                q = k % 2
                S.op("act", lambda e, k=k, q=q: e.activation(out=SQB[q], in_=H[:, k, c0:c0 + N], func=AF.Square),
                     reads=[bH[c]], writes=[bSQ[q]])
                S.op("pe", lambda e, k=k, q=q: e.matmul(psum[6][:, 0:N], lhsT=ones_b[:], rhs=SQB[q],
                                                        start=(k == 0), stop=(k == KC - 1)),
                     reads=[bSQ[q], bC], writes=[PB[6]])
            S.op("dve", lambda e: e.tensor_scalar(out=RS_[:], in0=psum[6][:, 0:N], scalar1=1.0 / D, scalar2=RMS_EPS,
                                                  op0=ALU.mult, op1=ALU.add), reads=[PB[6]], writes=[bRS_])
            S.op("act", lambda e: e.activation(out=RS_[:], in_=RS_[:], func=AF.Sqrt), reads=[bRS_], writes=[bRS_])
            S.op("dve", lambda e: e.reciprocal(out=RS_[:], in_=RS_[:]), reads=[bRS_], writes=[bRS_])

        def norm_to_xn(c, ri, rsx=None):
            c0 = c * N
            RS_, bRS_ = (RS[ri], bRS[ri]) if rsx is None else rsx
            S.op("dve", lambda e: e.tensor_tensor(out=XN[:, :, c0:c0 + N], in0=H[:, :, c0:c0 + N],
                                                  in1=bc(RS_[:], KC), op=ALU.mult),
                 reads=[bH[c], bRS_], writes=[bXN[c]])

        def dump_dbg():
            t = S.dma("sp", lambda e: e.dma_start(out=dbg_out[:, :, :], in_=H[:]), dbg_sem, reads=bH)
            return t

        def phase_load(s):
            scr.reset()
            bar = S.barrier()
            NXT = 4
            XT = [scr.f32([128, D]) for _ in range(NXT)]
            bXT = [Buf(f"xt{i}", bar) for i in range(NXT)]
            ntile = (L + 127) // 128
            tcount = 0
            for i in range(ntile):
                t0 = i * 128
                nt = min(128, L - t0)
                q = i % NXT
                if i == 0:
                    S.dma("sp", lambda e, q=q: e.dma_start(out=XT[q][0:NMETA, :], in_=dr["meta_tokens"][:, :]),
                          in_sem[q], writes=[bXT[q]])
                    S.dma("sp", lambda e, q=q: e.dma_start(out=XT[q][NMETA:128, :], in_=dr["x"][s, 0:128 - NMETA, :]),
                          in_sem[q], writes=[])
                    bXT[q].w = {in_sem[q][0]: in_sem[q][1]}
                else:
                    S.dma("sp", lambda e, q=q, t0=t0, nt=nt: e.dma_start(out=XT[q][0:nt, :],
                                                                          in_=dr["x"][s, t0 - NMETA:t0 - NMETA + nt, :]),
                          in_sem[q], writes=[bXT[q]])
                for k in range(KC):
                    pb = tcount % 4
                    tcount += 1
                    S.op("pe", lambda e, q=q, k=k, nt=nt, pb=pb: e.transpose(out=psum[pb][:, 0:nt],
                                                                              in_=XT[q][0:nt, k * 128:(k + 1) * 128],
                                                                              identity=ident_f[0:nt, 0:nt]),
                         reads=[bXT[q], bC], writes=[PB[pb]])
                    if k % 2 == 0:
                        S.op("dve", lambda e, k=k, t0=t0, nt=nt, pb=pb: e.tensor_copy(out=H[:, k, t0:t0 + nt],
                                                                                      in_=psum[pb][:, 0:nt]),
                             reads=[PB[pb]], writes=Hb(t0, t0 + nt))
                    else:
                        S.op("act", lambda e, k=k, t0=t0, nt=nt, pb=pb: e.copy(out=H[:, k, t0:t0 + nt],
                                                                               in_=psum[pb][:, 0:nt]),
                             reads=[PB[pb]], writes=Hb(t0, t0 + nt))

        def conv_units():
            w_in = dr["a_w_in"][0]
            w_out = dr["a_w_out"][0]
            ids = []
            for hf in range(2):
                for j in (2 * hf, 2 * hf + 1):
                    ids.append(("b", j, unitA(w_in, 256 * j, G_A)))
                    ids.append(("c", j, unitA(w_in, 1024 + 256 * j, G_A)))
                    ids.append(("u", j, unitA(w_in, 2048 + 256 * j, G_A)))
                ids.append(("o", hf, unitB(w_out, 512 * hf, 0)))
                ids.append(("o2", hf, unitB(w_out, 512 * hf, 512)))
            return ids

        def phase_conv(ids):
            scr.reset()
            bar = S.barrier()
            Z = scr.f32([128, L + 2])
            CS = [scr.f32([128, N]) for _ in range(2)]
            CV = [scr.f32([128, N]) for _ in range(2)]
            BZ = scr.bf16([128, 4, L])
            bZ = [Buf(f"z{c}", bar) for c in range(NCHUNK)]
            bZ0 = Buf("zpad", bar)
            bCS = [Buf("cs0", bar), Buf("cs1", bar)]
            bCV = [Buf("cv0", bar), Buf("cv1", bar)]
            bBZ = [Buf(f"bz{c}", bar) for c in range(NCHUNK)]
            um = {(n, j): u for n, j, u in ids}
            for c in range(NCHUNK):
                norm_stats(c, c % 2)
                norm_to_xn(c, c % 2)
            S.op("pool", lambda e: e.memset(Z[:, 0:2], 0.0), writes=[bZ0])
            it = 0
            for hf in range(2):
                for j in (2 * hf, 2 * hf + 1):
                    ub, uc, uu = um[("b", j)], um[("c", j)], um[("u", j)]
                    ensure_cast(uu + 2)
                    vb, vc, vu = ring_view(ub), ring_view(uc), ring_view(uu)
                    for mm in range(2):
                        m = 2 * j + mm
                        ml = m - 4 * hf
                        for c in range(NCHUNK):
                            c0 = c * N
                            q = it % 2
                            it += 1
                            pb_, pc_, pu_ = q, 2 + q, 4 + q
                            cols = slice(mm * 128, (mm + 1) * 128)
                            mm_group(psum[pc_][:, 0:N], [(vc[:, k, cols], XN[:, k, c0:c0 + N]) for k in range(KC)],
                                     reads=[rb(uc), bXN[c]], writes=[PB[pc_]])
                            mm_group(psum[pu_][:, 0:N], [(vu[:, k, cols], XN[:, k, c0:c0 + N]) for k in range(KC)],
                                     reads=[rb(uu), bXN[c]], writes=[PB[pu_]])
                            mm_group(psum[pb_][:, 0:N], [(vb[:, k, cols], XN[:, k, c0:c0 + N]) for k in range(KC)],
                                     reads=[rb(ub), bXN[c]], writes=[PB[pb_]])
                            S.op("act", lambda e, q=q, pc_=pc_: e.copy(out=CS[q][:], in_=psum[pc_][:, 0:N]),
                                 reads=[PB[pc_]], writes=[bCS[q]])
                            S.op("dve", lambda e, q=q, pu_=pu_, c0=c0: e.tensor_tensor(out=Z[:, 2 + c0:2 + c0 + N],
                                                                                       in0=psum[pu_][:, 0:N], in1=CS[q][:],
                                                                                       op=ALU.mult),
                                 reads=[PB[pu_], bCS[q]], writes=[bZ[c]])
                            zr = [bZ[c], bZ[c - 1] if c > 0 else bZ0]
                            S.op("act", lambda e, q=q, c0=c0, m=m: e.activation(
                                out=CV[q][:], in_=Z[:, 2 + c0:2 + c0 + N], func=AF.Copy, scale=CW[:, 2, m:m + 1]),
                                reads=zr + [bC], writes=[bCV[q]])
                            S.op("dve", lambda e, q=q, c0=c0, m=m: e.scalar_tensor_tensor(
                                out=CV[q][:], in0=Z[:, 1 + c0:1 + c0 + N], scalar=CW[:, 1, m:m + 1], in1=CV[q][:],
                                op0=ALU.mult, op1=ALU.add), reads=zr + [bC], writes=[bCV[q]])
                            S.op("dve", lambda e, q=q, c0=c0, m=m: e.scalar_tensor_tensor(
                                out=CV[q][:], in0=Z[:, c0:c0 + N], scalar=CW[:, 0, m:m + 1], in1=CV[q][:],
                                op0=ALU.mult, op1=ALU.add), reads=zr + [bC], writes=[bCV[q]])
                            S.op("dve", lambda e, q=q, pb_=pb_, c0=c0, ml=ml: e.tensor_tensor(
                                out=BZ[:, ml, c0:c0 + N], in0=psum[pb_][:, 0:N], in1=CV[q][:], op=ALU.mult),
                                reads=[PB[pb_], bCV[q]], writes=[bBZ[c]])
                uo = [um[("o", hf)], um[("o2", hf)]]
                ensure_cast(uo[1] + 2)
                for o in range(KC):
                    vo = ring_view(uo[o // 4])
                    cols = slice((o % 4) * 128, (o % 4 + 1) * 128)
                    for c in range(NCHUNK):
                        c0 = c * N
                        pd = (6, 7, 0, 1)[it % 4]
                        it += 1
                        mm_group(psum[pd][:, 0:N], [(vo[:, k, cols], BZ[:, k, c0:c0 + N]) for k in range(4)],
                                 reads=[rb(uo[o // 4]), bBZ[c]], writes=[PB[pd]])
                        S.op("dve", lambda e, o=o, c0=c0, pd=pd: e.tensor_tensor(out=H[:, o, c0:c0 + N],
                                                                                 in0=psum[pd][:, 0:N],
                                                                                 in1=H[:, o, c0:c0 + N], op=ALU.add),
                             reads=[PB[pd]], writes=[bH[c]])

        def moe_units(layer):
            ids = []
            for ex in range(NEXP):
                wg = dr["expert_w_gate"][layer, ex]
                wu = dr["expert_w_up"][layer, ex]
                wd = dr["expert_w_down"][layer, ex]
                gi = G_F0 + layer
                u0 = unitA(wg, 0, gi)
                unitA(wg, 256, gi)
                unitA(wu, 0, gi)
                unitA(wu, 256, gi)
                unitB(wd, 0, 0)
                unitB(wd, 0, 512)
                ids.append(u0)
            return ids

        def phase_moe(layer, ids):
            scr.reset()
            bar = S.barrier()
            HE = [scr.bf16([128, 4, N]) for _ in range(2)]
            SIL = [scr.f32([128, N]) for _ in range(2)]
            CB = [scr.f32([128, N]) for _ in range(2)]
            CT = scr.bf16([128, L])
            XR = [scr.f32([128, N]) for _ in range(2)]
            LGT = [scr.f32([20, N]) for _ in range(2)]
            NT = 18
            LG = scr.f32([128, NT, 20])
            LEM = scr.f32([128, NT, 16])
            OH1 = scr.f32([128, NT, 16])
            LEM2 = scr.f32([128, NT, 16])
            OH2 = scr.f32([128, NT, 16])
            CMB = scr.f32([128, NT, 16])
            OHG = scr.f32([128, NT, 4])
            EG = scr.f32([128, NT, 4])
            PEN = scr.f32([128, NT, 4])
            SC = scr.f32([128, 8, NT])
            bHE = [Buf("he0", bar), Buf("he1", bar)]
            bSIL = [Buf("sil0", bar), Buf("sil1", bar)]
            bCB = [Buf("cb0", bar), Buf("cb1", bar)]
            bCT = Buf("ct", bar)
            bXR = [Buf("xr0", bar), Buf("xr1", bar)]
            bLGT = [Buf("lgt0", bar), Buf("lgt1", bar)]
            bLG = Buf("lg", bar)
            bR = Buf("rt", bar)

            def topk(ta, tb):
                nT = tb - ta
                lg4 = LG[:, ta:tb, 0:4]
                le16 = LG[:, ta:tb, 4:20]
                MG, SE, PSEL, V1, V2, DD, W1, W2 = [SC[:, i, ta:tb] for i in range(8)]
                OHG_, EG_, PEN_ = OHG[:, ta:tb, :], EG[:, ta:tb, :], PEN[:, ta:tb, :]
                LEM_, OH1_, LEM2_, OH2_, CMB_ = (LEM[:, ta:tb, :], OH1[:, ta:tb, :], LEM2[:, ta:tb, :], OH2[:, ta:tb, :],
                                                 CMB[:, ta:tb, :])

                def dv(fn):
                    S.op("dve", fn, reads=[bR, bLG], writes=[bR])

                dv(lambda e: e.tensor_reduce(out=MG, in_=lg4, axis=AX.X, op=ALU.max))
                dv(lambda e: e.tensor_tensor(out=OHG_, in0=lg4, in1=bcl(MG, 4), op=ALU.is_equal))
                dv(lambda e: e.tensor_tensor(out=EG_, in0=lg4, in1=bcl(MG, 4), op=ALU.subtract))
                S.op("act", lambda e: e.activation(out=EG_, in_=EG_, func=AF.Exp), reads=[bR], writes=[bR])
                dv(lambda e: e.tensor_reduce(out=SE, in_=EG_, axis=AX.X, op=ALU.add))
                dv(lambda e: e.reciprocal(out=PSEL, in_=SE))
                dv(lambda e: e.tensor_scalar(out=PEN_, in0=OHG_, scalar1=-1.0, scalar2=BIG, op0=ALU.add, op1=ALU.mult))
                dv(lambda e: e.tensor_tensor(out=LEM_.rearrange("p t (g j) -> p t g j", g=4),
                                             in0=le16.rearrange("p t (g j) -> p t g j", g=4),
                                             in1=PEN_.unsqueeze(3).broadcast_to([128, nT, 4, 4]), op=ALU.add))
                dv(lambda e: e.tensor_reduce(out=V1, in_=LEM_, axis=AX.X, op=ALU.max))
                dv(lambda e: e.tensor_tensor(out=OH1_, in0=LEM_, in1=bcl(V1, 16), op=ALU.is_equal))
                dv(lambda e: e.scalar_tensor_tensor(out=LEM2_, in0=OH1_, scalar=-BIG, in1=LEM_, op0=ALU.mult, op1=ALU.add))
                dv(lambda e: e.tensor_reduce(out=V2, in_=LEM2_, axis=AX.X, op=ALU.max))
                dv(lambda e: e.tensor_tensor(out=OH2_, in0=LEM2_, in1=bcl(V2, 16), op=ALU.is_equal))
                dv(lambda e: e.tensor_tensor(out=DD, in0=V2, in1=V1, op=ALU.subtract))
                S.op("act", lambda e: e.activation(out=DD, in_=DD, func=AF.Exp), reads=[bR], writes=[bR])
                dv(lambda e: e.tensor_scalar(out=W1, in0=DD, scalar1=1.0, scalar2=None, op0=ALU.add))
                dv(lambda e: e.reciprocal(out=W1, in_=W1))
                dv(lambda e: e.tensor_tensor(out=W2, in0=DD, in1=W1, op=ALU.mult))
                dv(lambda e: e.tensor_tensor(out=W1, in0=W1, in1=PSEL, op=ALU.mult))
                dv(lambda e: e.tensor_tensor(out=W2, in0=W2, in1=PSEL, op=ALU.mult))
                dv(lambda e: e.tensor_tensor(out=OH1_, in0=OH1_, in1=bcl(W1, 16), op=ALU.mult))
                dv(lambda e: e.tensor_tensor(out=OH2_, in0=OH2_, in1=bcl(W2, 16), op=ALU.mult))
                dv(lambda e: e.tensor_tensor(out=CMB_, in0=OH1_, in1=OH2_, op=ALU.add))
                for ti in range(ta, tb):
                    t0, nt = tiles[ti]
                    pb = ti % 2
                    S.op("pe", lambda e, ti=ti, nt=nt, pb=pb: e.transpose(out=psum[pb][0:16, 0:nt], in_=CMB[0:nt, ti, :],
                                                                          identity=ident_f[0:nt, 0:nt]),
                         reads=[bR, bC], writes=[PB[pb]])
                    S.op("act", lambda e, t0=t0, nt=nt, pb=pb: e.copy(out=CT[0:16, t0:t0 + nt], in_=psum[pb][0:16, 0:nt]),
                         reads=[PB[pb]], writes=[bCT])

            S.op("pool", lambda e: e.memset(LG[:], 0.0), writes=[bLG])
            S.op("pool", lambda e: e.memset(CT[:], 0.0), writes=[bCT])
            tiles = []
            xi = 0
            rsx = [(RS[0], bRS[0]), (RS[1], bRS[1]), (SIL[0], bSIL[0]), (SIL[1], bSIL[1]), (CB[0], bCB[0]),
                   (CB[1], bCB[1])]
            for c in range(NCHUNK):
                norm_stats(c, None, rsx[c])
                norm_to_xn(c, None, rsx[c])
            for c in range(NCHUNK):
                c0 = c * N
                RSc, bRSc = rsx[c]
                for k in range(KC):
                    xq = xi % 2
                    xi += 1
                    S.op("dve" if k % 2 == 0 else "pool", lambda e, k=k, xq=xq, c0=c0, RSc=RSc: e.tensor_tensor(
                        out=XR[xq][:], in0=H[:, k, c0:c0 + N], in1=RSc[:], op=ALU.mult),
                        reads=[bH[c], bRSc], writes=[bXR[xq]])
                    S.op("pe", lambda e, k=k, xq=xq: e.matmul(psum[7][0:20, 0:N], lhsT=WRS[:, layer, k, :], rhs=XR[xq][:],
                                                              start=(k == 0), stop=(k == KC - 1)),
                         reads=[bXR[xq], bC], writes=[PB[7]])
                lq = c % 2
                S.op("act", lambda e, lq=lq: e.copy(out=LGT[lq][:], in_=psum[7][0:20, 0:N]), reads=[PB[7]],
                     writes=[bLGT[lq]])
                for (o, nt) in ((0, 128), (128, 128), (256, N - 256)):
                    ti = len(tiles)
                    t0 = c0 + o
                    tiles.append((t0, nt))
                    rbk = 5 if ti % 2 == 0 else 4
                    S.op("pe", lambda e, lq=lq, o=o, nt=nt, rbk=rbk: e.transpose(out=psum[rbk][0:nt, 0:20],
                                                                                 in_=LGT[lq][0:20, o:o + nt],
                                                                                 identity=ident_f[0:20, 0:20]),
                         reads=[bLGT[lq], bC], writes=[PB[rbk]])
                    S.op("dve", lambda e, ti=ti, nt=nt, rbk=rbk: e.tensor_tensor(out=LG[0:nt, ti, :], in0=psum[rbk][0:nt, 0:20],
                                                                                 in1=RB[0:nt, layer, :], op=ALU.add),
                         reads=[PB[rbk], bC], writes=[bLG])
            topk(0, 3 * NCHUNK)
            assert len(tiles) == NT
            it = 0

            def gate_up(ex, c, hq, ug, uu):
                nonlocal it
                c0 = c * N
                S.op("pe", lambda e: e.matmul(psum[0][:, 0:N], lhsT=SEL[:, ex, :], rhs=CT[:, c0:c0 + N],
                                              start=True, stop=True), reads=[bC, bCT], writes=[PB[0]])
                S.op("act", lambda e: e.copy(out=CB[hq][:], in_=psum[0][:, 0:N]), reads=[PB[0]], writes=[bCB[hq]])
                for m in range(4):
                    q = it % 2
                    it += 1
                    pg, pu = 2 + q, 4 + q
                    vg, vu = ring_view(ug[m // 2]), ring_view(uu[m // 2])
                    cols = slice((m % 2) * 128, (m % 2 + 1) * 128)
                    mm_group(psum[pg][:, 0:N], [(vg[:, k, cols], XN[:, k, c0:c0 + N]) for k in range(KC)],
                             reads=[rb(ug[m // 2]), bXN[c]], writes=[PB[pg]])
                    mm_group(psum[pu][:, 0:N], [(vu[:, k, cols], XN[:, k, c0:c0 + N]) for k in range(KC)],
                             reads=[rb(uu[m // 2]), bXN[c]], writes=[PB[pu]])
                    S.op("act", lambda e, q=q, pg=pg: e.activation(out=SIL[q][:], in_=psum[pg][:, 0:N], func=AF.Silu),
                         reads=[PB[pg]], writes=[bSIL[q]])
                    S.op("dve", lambda e, q=q, pu=pu: e.tensor_tensor(out=SIL[q][:], in0=psum[pu][:, 0:N],
                                                                      in1=SIL[q][:], op=ALU.mult),
                         reads=[PB[pu], bSIL[q]], writes=[bSIL[q]])
                    S.op("pool", lambda e, q=q, m=m: e.tensor_tensor(out=HE[hq][:, m, :], in0=SIL[q][:],
                                                                     in1=CB[hq][:], op=ALU.mult),
                         reads=[bSIL[q], bCB[hq]], writes=[bHE[hq]])

            dcnt = [0]

            def down(c, hq, ud):
                c0 = c * N
                for o in range(KC):
                    pd = (1, 6, 7)[dcnt[0] % 3]
                    dcnt[0] += 1
                    vd = ring_view(ud[o // 4])
                    cols = slice((o % 4) * 128, (o % 4 + 1) * 128)
                    mm_group(psum[pd][:, 0:N], [(vd[:, k, cols], HE[hq][:, k, :]) for k in range(4)],
                             reads=[rb(ud[o // 4]), bHE[hq]], writes=[PB[pd]])
                    S.op("dve", lambda e, o=o, pd=pd: e.tensor_tensor(out=H[:, o, c0:c0 + N], in0=psum[pd][:, 0:N],
                                                                      in1=H[:, o, c0:c0 + N], op=ALU.add),
                         reads=[PB[pd]], writes=[bH[c]])

            prev = None
            idx = 0
            for ex in range(NEXP):
                u0 = ids[ex]
                ug, uu, ud = (u0, u0 + 1), (u0 + 2, u0 + 3), (u0 + 4, u0 + 5)
                ensure_cast(u0 + 5)
                for c in range(NCHUNK):
                    ensure_cast(u0 + 6 + c)
                    hq = idx % 2
                    idx += 1
                    gate_up(ex, c, hq, ug, uu)
                    if prev is not None:
                        down(*prev)
                    prev = (c, hq, ud)
            down(*prev)

        def attn_units():
            ids = []
            wq = dr["b_w_q"][0]
            wkv = dr["w_kv"]
            wo = dr["b_w_o"][0]
            for j in range(4):
                uq = unitA(wq, 256 * j, G_B)
                unitA(wkv, 256 * j, G_KV)
                unitA(wkv, 1024 + 256 * j, G_KV)
                unitC(wo, 256 * j)
                ids.append(uq)
            return ids

        def phase_attn(ids):
            scr.reset()
            bar = S.barrier()
            QW = 384
            NPT = 4
            SKEW = 2
            QT = [scr.bf16([128, L]) for _ in range(2)]
            KT = scr.bf16([128, L])
            VA = scr.bf16([128, 17, 128])
            OT = scr.bf16([128, L])
            PT = [scr.bf16([128, QW]) for _ in range(NPT)]
            R0 = scr.f32([128, QW])
            R1 = scr.f32([128, QW])
            OA = scr.f32([128, QW])
            OB2 = scr.f32([128, QW])
            SQb = scr.bf16([128, QW])
            RSTD = scr.f32([128, QW])
            bQT, bKT, bVA = Buf("qt", bar), Buf("kt", bar), Buf("va", bar)
            bOT = [Buf(f"ot{c}", bar) for c in range(NCHUNK)]
            bPT = [Buf(f"pt{i}", bar) for i in range(NPT)]
            bE = Buf("epi", bar)
            bHT = [Buf("ht0", bar), Buf("ht1", bar)]
            SB_ = (0, 1, 2)
            OBK = (3, 5)
            SBK = (4, 6)
            MB_ = (7, 5, 6)

            for c in range(NCHUNK):
                norm_stats(c, c % 2)
                norm_to_xn(c, c % 2)
            S.op("pool", lambda e: e.memset(QT[0][64:128, :], 0.0), writes=[bQT])
            S.op("pool", lambda e: e.memset(QT[1][0:64, :], 0.0), writes=[bQT])
            qchunks = []
            q = 0
            while q < L:
                qchunks.append((q, min(QW, L - q)))
                q += QW
            cnt = dict(s=0, pt=0, mb=0, ep=0)
            deferred = []
            pending_tail = []
            DEFER = 8

            def mbank():
                b = MB_[cnt["mb"] % 3]
                cnt["mb"] += 1
                return b

            for j in range(4):
                uq, uk, uv, uo = ids[j], ids[j] + 1, ids[j] + 2, ids[j] + 3
                ensure_cast(uo + 2)
                vq, vk, vv, vo = ring_view(uq), ring_view(uk), ring_view(uv), ring_view(uo)
                for hh in range(2):
                    cols = slice(hh * 128, (hh + 1) * 128)
                    for c in range(NCHUNK):
                        c0 = c * N
                        pq = mbank()
                        mm_group(psum[pq][:, 0:N], [(vq[:, k, cols], XN[:, k, c0:c0 + N]) for k in range(KC)],
                                 reads=[rb(uq), bXN[c]], writes=[PB[pq]])
                        S.op("act", lambda e, c0=c0, pq=pq: e.activation(out=QT[0][0:64, c0:c0 + N], in_=psum[pq][0:64, 0:N],
                                                                         func=AF.Copy, scale=0.125),
                             reads=[PB[pq]], writes=[bQT])
                        S.op("act", lambda e, c0=c0, pq=pq: e.activation(out=QT[1][64:128, c0:c0 + N],
                                                                         in_=psum[pq][64:128, 0:N], func=AF.Copy, scale=0.125),
                             reads=[PB[pq]], writes=[bQT])
                        pq = mbank()
                        mm_group(psum[pq][:, 0:N], [(vk[:, k, cols], XN[:, k, c0:c0 + N]) for k in range(KC)],
                                 reads=[rb(uk), bXN[c]], writes=[PB[pq]])
                        S.op("dve", lambda e, c0=c0, pq=pq: e.tensor_copy(out=KT[:, c0:c0 + N], in_=psum[pq][:, 0:N]),
                             reads=[PB[pq]], writes=[bKT])
                    for t in range(17):
                        t0 = t * 128
                        nt = min(128, L - t0)
                        pq = mbank()
                        mm_group(psum[pq][0:nt, 0:128], [(XN[:, k, t0:t0 + nt], vv[:, k, cols]) for k in range(KC)],
                                 reads=[rb(uv)] + XNb(t0, t0 + nt), writes=[PB[pq]])
                        if t % 2 == 0:
                            S.op("dve", lambda e, t=t, nt=nt, pq=pq: e.tensor_copy(out=VA[0:nt, t, 0:128],
                                                                                   in_=psum[pq][0:nt, 0:128]),
                                 reads=[PB[pq]], writes=[bVA])
                        else:
                            S.op("act", lambda e, t=t, nt=nt, pq=pq: e.copy(out=VA[0:nt, t, 0:128],
                                                                            in_=psum[pq][0:nt, 0:128]),
                                 reads=[PB[pq]], writes=[bVA])

                    while pending_tail:
                        pending_tail.pop(0)()
                    def stageA(step):
                        (q0, nq, cc, kt, last, kt_max) = step
                        q1 = q0 + nq
                        rows = slice(cc * 64, (cc + 1) * 64)
                        k0 = kt * 128
                        nk = min(128, L - k0)
                        qlo = max(q0, k0)
                        width = q1 - qlo
                        sb_ = SB_[cnt["s"] % 3]
                        cnt["s"] += 1
                        pi = cnt["pt"] % NPT
                        cnt["pt"] += 1
                        diag = k0 >= q0
                        dw = min(128, width)

                        def qk(e):
                            ins = e.matmul(psum[sb_][0:nk, 0:width], lhsT=KT[:, k0:k0 + nk], rhs=QT[cc][:, qlo:q1],
                                           start=True, stop=not diag)
                            if diag:
                                ins = e.matmul(psum[sb_][0:nk, 0:dw], lhsT=ident_b[0:nk, 0:nk], rhs=maskneg[0:nk, 0:dw],
                                               start=False, stop=True)
                            return ins
                        S.op("pe", qk, reads=[bKT, bQT, bC], writes=[PB[sb_]])
                        S.op("act", lambda e: e.activation(out=PT[pi][0:nk, 0:width], in_=psum[sb_][0:nk, 0:width],
                                                           func=AF.Exp), reads=[PB[sb_]], writes=[bPT[pi]])
                        return (pi, nk, qlo)

                    def stageB(step, a):
                        (q0, nq, cc, kt, last, kt_max) = step
                        (pi, nk, qlo) = a
                        off = qlo - q0
                        width = q0 + nq - qlo

                        def pv(e):
                            e.matmul(psum[OBK[cc]][:, off:nq], lhsT=VA[0:nk, kt, :], rhs=PT[pi][0:nk, 0:width],
                                     start=(kt == 0), stop=(kt == kt_max))
                            return e.matmul(psum[SBK[cc]][:, off:nq], lhsT=ones_b[0:nk, :], rhs=PT[pi][0:nk, 0:width],
                                            start=(kt == 0), stop=(kt == kt_max))
                        S.op("pe", pv, reads=[bPT[pi], bVA, bC], writes=[PB[OBK[cc]], PB[SBK[cc]]])
                        if last:
                            epilogue(q0, nq)

                    def epilogue(q0, nq):
                        q1 = q0 + nq
                        rd, wr = [bE, bC], [bE]
                        o0, s0 = psum[OBK[0]][:, 0:nq], psum[SBK[0]][:, 0:nq]
                        o1, s1 = psum[OBK[1]][:, 0:nq], psum[SBK[1]][:, 0:nq]
                        S.op("dve", lambda e: e.tensor_copy(out=R0[:, 0:nq], in_=s0), reads=rd + [PB[SBK[0]]], writes=wr)
                        S.op("dve", lambda e: e.tensor_copy(out=OA[:, 0:nq], in_=o0), reads=rd + [PB[OBK[0]]], writes=wr)
                        S.op("dve", lambda e: e.tensor_copy(out=R1[:, 0:nq], in_=s1), reads=rd + [PB[SBK[1]]], writes=wr)
                        S.op("dve", lambda e: e.tensor_copy(out=OB2[:, 0:nq], in_=o1), reads=rd + [PB[OBK[1]]], writes=wr)
                        S.op("dve", lambda e: e.reciprocal(out=R0[:, 0:nq], in_=R0[:, 0:nq]), reads=rd, writes=wr)
                        S.op("dve", lambda e: e.reciprocal(out=R1[:, 0:nq], in_=R1[:, 0:nq]), reads=rd, writes=wr)
                        S.op("dve", lambda e: e.tensor_tensor(out=OA[:, 0:nq], in0=OA[:, 0:nq], in1=R0[:, 0:nq], op=ALU.mult),
                             reads=rd, writes=wr)
                        S.op("dve", lambda e: e.scalar_tensor_tensor(out=OB2[:, 0:nq], in0=OB2[:, 0:nq], scalar=NEGLAM[:, 0:1],
                                                                     in1=R1[:, 0:nq], op0=ALU.mult, op1=ALU.mult),
                             reads=rd, writes=wr)
                        S.op("pool", lambda e: e.tensor_tensor(out=OT[:, q0:q1], in0=OA[:, 0:nq], in1=OB2[:, 0:nq], op=ALU.add),
                             reads=rd, writes=[bE] + [bOT[cx] for cx in chunks_of(q0, q1)])

                    steps = []
                    for (q0, nq) in qchunks:
                        kt_max = (q0 + nq - 1) // 128
                        for cc in range(2):
                            for kt in range(kt_max + 1):
                                steps.append((q0, nq, cc, kt, cc == 1 and kt == kt_max, kt_max))
                    def tick():
                        for d in deferred:
                            d[0] -= 1
                        while deferred and deferred[0][0] <= 0:
                            deferred.pop(0)[1]()

                    pend = []
                    for st_ in steps:
                        pend.append((st_, stageA(st_)))
                        if len(pend) > SKEW:
                            s0, a0 = pend.pop(0)
                            stageB(s0, a0)
                        tick()
                    while pend:
                        s0, a0 = pend.pop(0)
                        stageB(s0, a0)
                        tick()

                    def head_tail(hh=hh, vo=vo, uo=uo):
                        while deferred:
                            deferred.pop(0)[1]()
                        sq_t = [SQb[:, 0:N], OA[:, 0:N // 2].bitcast(BF16)]
                        rs_t = [R0[:, 0:N], R1[:, 0:N]]
                        for c in range(NCHUNK):
                            c0 = c * N
                            par = c % 2
                            sqv, rsv, bt = sq_t[par], rs_t[par], bHT[par]
                            S.op("pool", lambda e, c0=c0, sqv=sqv: e.tensor_tensor(out=sqv, in0=OT[:, c0:c0 + N],
                                                                                  in1=OT[:, c0:c0 + N], op=ALU.mult),
                                 reads=[bOT[c], bE], writes=[bt])
                            pq = mbank()
                            S.op("pe", lambda e, pq=pq, sqv=sqv: e.matmul(psum[pq][:, 0:N], lhsT=ones_b[:], rhs=sqv, start=True,
                                                                          stop=True), reads=[bt, bC, bE], writes=[PB[pq]])
                            S.op("dve", lambda e, pq=pq, rsv=rsv: e.tensor_scalar(out=rsv, in0=psum[pq][:, 0:N], scalar1=1.0 / 128,
                                                                                  scalar2=SUBLN_EPS, op0=ALU.mult, op1=ALU.add),
                                 reads=[PB[pq], bE], writes=[bt])
                            S.op("act", lambda e, rsv=rsv: e.activation(out=rsv, in_=rsv, func=AF.Ln), reads=[bt, bE], writes=[bt])
                            S.op("act", lambda e, rsv=rsv: e.activation(out=rsv, in_=rsv, func=AF.Exp, scale=-0.5),
                                 reads=[bt, bE], writes=[bt])
                            S.op("dve", lambda e, c0=c0, rsv=rsv: e.scalar_tensor_tensor(
                                out=OT[:, c0:c0 + N], in0=OT[:, c0:c0 + N], scalar=GSC[:, 0:1], in1=rsv, op0=ALU.mult,
                                op1=ALU.mult), reads=[bt, bE, bC, bOT[c]], writes=[bt, bOT[c]])
                        for o in range(KC):
                            for c in range(NCHUNK):
                                c0 = c * N
                                pq = mbank()
                                S.op("pe", lambda e, o=o, c0=c0, pq=pq: e.matmul(
                                    psum[pq][:, 0:N], lhsT=vo[:, hh, o * 128:(o + 1) * 128], rhs=OT[:, c0:c0 + N],
                                    start=True, stop=True), reads=[rb(uo), bOT[c]], writes=[PB[pq]])
                                S.op("dve", lambda e, o=o, c0=c0, pq=pq: e.tensor_tensor(out=H[:, o, c0:c0 + N],
                                                                                         in0=psum[pq][:, 0:N],
                                                                                         in1=H[:, o, c0:c0 + N], op=ALU.add),
                                     reads=[PB[pq]], writes=[bH[c]])
                    pending_tail.append(head_tail)
            while pending_tail:
                pending_tail.pop(0)()

        def phase_final(s):
            scr.reset()
            bar = S.barrier()
            Y = [scr.f32([128, N]) for _ in range(3)]
            OUTT = [scr.f32([128, D]) for _ in range(6)]
            bY = [Buf("y0", bar), Buf("y1", bar), Buf("y2", bar)]
            bOUT = [Buf(f"outt{i}", bar) for i in range(6)]
            toks = []
            yi = 0
            pj = 0
            norm_stats(0, 0)
            for c in range(NCHUNK):
                ri = c % 2
                c0 = c * N
                if c + 1 < NCHUNK:
                    norm_stats(c + 1, (c + 1) % 2)
                subs = ((0, 128), (128, 128), (256, N - 256))
                for k in range(KC):
                    q = yi % 3
                    yi += 1
                    S.op("dve", lambda e, q=q, k=k, c0=c0, ri=ri: e.scalar_tensor_tensor(
                        out=Y[q][:], in0=H[:, k, c0:c0 + N], scalar=G[:, G_FIN, k:k + 1], in1=RS[ri][:],
                        op0=ALU.mult, op1=ALU.mult), reads=[bH[c], bRS[ri], bC], writes=[bY[q]])
                    for si0, (o, nt) in enumerate(subs):
                        si = (c % 2) * 3 + si0
                        pq = pj % 4
                        pj += 1
                        S.op("pe", lambda e, q=q, o=o, nt=nt, pq=pq: e.transpose(out=psum[pq][0:nt, 0:128],
                                                                                 in_=Y[q][:, o:o + nt], identity=ident_f[:]),
                             reads=[bY[q], bC], writes=[PB[pq]])
                        if (k + si0) % 2 == 0:
                            S.op("act", lambda e, si=si, k=k, nt=nt, pq=pq: e.copy(out=OUTT[si][0:nt, k * 128:(k + 1) * 128],
                                                                                   in_=psum[pq][0:nt, 0:128]),
                                 reads=[PB[pq]], writes=[bOUT[si]])
                        else:
                            S.op("dve", lambda e, si=si, k=k, nt=nt, pq=pq: e.tensor_copy(
                                out=OUTT[si][0:nt, k * 128:(k + 1) * 128], in_=psum[pq][0:nt, 0:128]),
                                reads=[PB[pq]], writes=[bOUT[si]])
                for si0, (o, nt) in enumerate(subs):
                    si = (c % 2) * 3 + si0
                    t0 = c0 + o
                    lo = max(t0, NMETA)
                    if lo >= t0 + nt:
                        continue
                    p0 = lo - t0
                    toks.append(S.dma("sp", lambda e, si=si, p0=p0, nt=nt, lo=lo, t0=t0: e.dma_start(
                        out=out[s, lo - NMETA:t0 + nt - NMETA, :], in_=OUTT[si][p0:nt, :]), out_sem[si], reads=[bOUT[si]]))
            return toks

        consts()
        out_toks = []
        ulists = []
        for s in range(nseq):
            ulists.append((conv_units(), moe_units(0), attn_units(), moe_units(1)))
        ensure_cast(4)
        for s in range(nseq):
            if s > 0:
                S.epoch()
            cu, m0, au, m1 = ulists[s]
            phase_load(s)
            done = stop_after == "load"
            if not done:
                phase_conv(cu)
                done = stop_after == "conv"
            if not done:
                phase_moe(0, m0)
                done = stop_after == "moe0"
            if not done:
                phase_attn(au)
                done = stop_after == "attn"
            if not done:
                phase_moe(1, m1)
                done = stop_after == "moe1"
            if dbg and s == 0:
                out_toks.append(dump_dbg())
            if done:
                break
            out_toks += phase_final(s)
        S.wait_only("sp", out_toks)
        S.replay()
    return nc


_NC_CACHE = {}


def kernel(**inputs):
    n_cores = 8
    if "nc" not in _NC_CACHE:
        _NC_CACHE["nc"] = build_nc()
    nc = _NC_CACHE["nc"]
    x = np.ascontiguousarray(inputs["x"], dtype=np.float32)
    shared = {}
    for name, shape in INPUT_SPECS:
        if name == "x":
            continue
        shared[name] = np.ascontiguousarray(np.asarray(inputs[name], dtype=np.float32).reshape(shape))
    in_maps = []
    for c in range(n_cores):
        m = dict(shared)
        m["x"] = np.ascontiguousarray(x[c * NSEQ:(c + 1) * NSEQ])
        in_maps.append(m)
    res = run_bass_kernel_spmd(nc, in_maps, core_ids=list(range(n_cores)))
    outs = [np.asarray(r["out"]) for r in res.results]
    return np.concatenate(outs, axis=0).astype(np.float32)
```

```python
import math
from contextlib import ExitStack

import numpy as np
import concourse.bass as bass
import concourse.mybir as mybir
from concourse.bass_utils import run_bass_kernel_spmd

F32 = mybir.dt.float32
BF16 = mybir.dt.bfloat16
ALU = mybir.AluOpType
AF = mybir.ActivationFunctionType
AX = mybir.AxisListType

D = 1024
KC = 8
SEQ = 2048
NMETA = 16
L = SEQ + NMETA
NCHUNK = 6
N = L // NCHUNK
NSEQ = 2
NEXP = 16
RMS_EPS = 1e-6
SUBLN_EPS = 1e-5
LAMBDA_INIT = 0.8 - 0.6 * math.exp(-0.3 * 1)
BIG = 1.0e30
NRING = 12
NSTG = 2
UNIT = 2048


class Buf:
    __slots__ = ("w", "r", "name")

    def __init__(self, name="", init=None):
        self.w = {}
        self.r = dict(init) if init else {}
        self.name = name


def _merge(dst, src):
    for s, v in src.items():
        if dst.get(s, 0) < v:
            dst[s] = v


class Sched:
    ENGS = ("pe", "act", "dve", "pool", "sp")

    def __init__(self, nc, stack):
        self.nc = nc
        self.stack = stack
        self.nsem = 0
        self.e = {}
        self.dma_slots = []
        self.old = {}
        for n in self.ENGS:
            self.e[n] = dict(ops=[], sem=None, cnt=0, seen={})
        self.epoch()

    def new_sem(self, name):
        s = self.stack.enter_context(self.nc.semaphore(f"{name}{self.nsem}"))
        self.nsem += 1
        return s

    def dma_slot(self, name):
        sl = [self.new_sem(name), 0]
        self.dma_slots.append(sl)
        return sl

    def epoch(self):
        for n in ("pe", "act", "dve", "pool"):
            if self.e[n]["sem"] is not None and self.e[n]["cnt"] > 0:
                self.old[self.e[n]["sem"]] = self.e[n]["cnt"]
            self.e[n]["sem"] = self.new_sem("e" + n)
            self.e[n]["cnt"] = 0

    def barrier(self):
        t = dict(self.old)
        for n in ("pe", "act", "dve", "pool"):
            if self.e[n]["cnt"] > 0:
                t[self.e[n]["sem"]] = self.e[n]["cnt"]
        for sl in self.dma_slots:
            if sl[1] > 0:
                t[sl[0]] = sl[1]
        return t

    def _waits(self, eng, reads, writes, extra):
        waits = {}
        for b in reads:
            _merge(waits, b.w)
        for b in writes:
            _merge(waits, b.w)
            _merge(waits, b.r)
        for t in extra:
            if t is not None:
                _merge(waits, {t[0]: t[1]})
        e = self.e[eng]
        need = []
        for s, v in waits.items():
            if eng == "pe" and s is e["sem"]:
                continue
            if e["seen"].get(s, 0) < v:
                e["seen"][s] = v
                need.append((s, v))
        return need

    def op(self, eng, fn, reads=(), writes=(), extra=()):
        e = self.e[eng]
        need = self._waits(eng, reads, writes, extra)
        e["cnt"] += 1
        tok = (e["sem"], e["cnt"])
        e["ops"].append((need, fn, tok, 1))
        for b in reads:
            _merge(b.r, {tok[0]: tok[1]})
        for b in writes:
            b.w = {tok[0]: tok[1]}
            b.r = {}
        return tok

    def dma(self, queue, fn, semslot, reads=(), writes=(), extra=()):
        e = self.e[queue]
        need = self._waits(queue, reads, writes, extra)
        if semslot[1] > 0 and e["seen"].get(semslot[0], 0) < semslot[1]:
            e["seen"][semslot[0]] = semslot[1]
            need.append((semslot[0], semslot[1]))
        semslot[1] += 16
        tok = (semslot[0], semslot[1])
        e["ops"].append((need, fn, tok, 16))
        for b in reads:
            _merge(b.r, {tok[0]: tok[1]})
        for b in writes:
            b.w = {tok[0]: tok[1]}
            b.r = {}
        return tok

    def wait_only(self, eng, toks):
        need = self._waits(eng, (), (), toks)
        self.e[eng]["ops"].append((need, None, None, 0))

    def replay(self):
        nc = self.nc
        sched = self

        def run(name, eng):
            for need, fn, tok, incv in sched.e[name]["ops"]:
                for s, v in need:
                    eng.wait_ge(s, v)
                if fn is None:
                    continue
                ins = fn(eng)
                ins.then_inc(tok[0], incv)

        with nc.Block() as block:
            @block.tensor
            def _(eng):
                run("pe", eng)

            @block.scalar
            def _(eng):
                run("act", eng)

            @block.vector
            def _(eng):
                run("dve", eng)

            @block.gpsimd
            def _(eng):
                run("pool", eng)

            @block.sync
            def _(eng):
                run("sp", eng)


INPUT_SPECS = [
    ("x", [NSEQ, SEQ, D]), ("meta_tokens", [NMETA, D]), ("a_norm", [1, D]), ("a_w_in", [1, D, 3 * D]),
    ("a_conv", [1, 3, D]), ("a_w_out", [1, D, D]), ("kv_norm", [D]), ("w_kv", [D, 2 * D]),
    ("b_norm", [1, D]), ("b_w_q", [1, D, D]), ("b_lambda", [1, 4, 64]), ("b_subln", [1, 128]),
    ("b_w_o", [1, D, D]), ("ffn_norm", [2, D]), ("router_group_w", [2, D, 4]), ("router_group_b", [2, 4]),
    ("router_expert_w", [2, D, 16]), ("router_expert_b", [2, 16]), ("expert_w_gate", [2, 16, D, 512]),
    ("expert_w_up", [2, 16, D, 512]), ("expert_w_down", [2, 16, 512, D]), ("final_norm", [D]),
]

G_A, G_F0, G_F1, G_KV, G_B, G_FIN = range(6)


def build_nc(nseq=NSEQ, stop_after=None, dbg=False):
    nc = bass.Bass("TRN2", target_bir_lowering=False)
    dr = {}
    for name, shape in INPUT_SPECS:
        dr[name] = nc.dram_tensor(name, shape, F32, kind="ExternalInput").ap()
    out = nc.dram_tensor("out", [NSEQ, SEQ, D], F32, kind="ExternalOutput").ap()
    dbg_out = None
    if dbg:
        dbg_out = nc.dram_tensor("dbg", [128, KC, L], F32, kind="ExternalOutput").ap()

    with ExitStack() as st:
        S = Sched(nc, st)

        def sb(name, shape, dt):
            return st.enter_context(nc.sbuf_tensor(name, shape, dt))

        H = sb("H", [128, KC, L], F32)
        XN = sb("XN", [128, KC, L], BF16)
        RING = sb("RING", [128, NRING, UNIT], BF16)
        STG = sb("STG", [128, NSTG, UNIT], F32)
        ident_f = sb("ident_f", [128, 128], F32)
        ident_b = sb("ident_b", [128, 128], BF16)
        mask01 = sb("mask01", [128, 128], BF16)
        maskneg = sb("maskneg", [128, 128], BF16)
        ones_f = sb("ones_f", [128, 128], F32)
        ones_b = sb("ones_b", [128, 128], BF16)
        SEL = sb("SEL", [128, 16, 128], BF16)
        G = sb("G", [128, 6, KC], F32)
        CW = sb("CW", [128, 3, KC], F32)
        WR32 = sb("WR32", [128, 2, KC, 20], F32)
        WRS = sb("WRS", [128, 2, KC, 20], F32)
        RB = sb("RB", [128, 2, 20], F32)
        GS = sb("GS", [128, 128], F32)
        GSC = sb("GSC", [128, 1], F32)
        NEGLAM = sb("NEGLAM", [128, 1], F32)
        LTMP = sb("LTMP", [128, 8], F32)
        SQ = [sb(f"SQ{i}", [128, N], F32) for i in range(2)]
        RS = [sb(f"RS{i}", [128, N], F32) for i in range(2)]
        SQB = [SQ[i][:, 0:N // 2].bitcast(BF16) for i in range(2)]
        SCR_BYTES = (nc.sbuf_bytes_remaining // 64) * 64 - 256
        SCR = sb("SCR", [128, SCR_BYTES // 4], F32)

        psum = [st.enter_context(nc.psum_tensor(f"ps{i}", [128, 512], F32)) for i in range(8)]
        PB = [Buf(f"ps{i}") for i in range(8)]

        class Scratch:
            def __init__(self):
                self.off = 0

            def reset(self):
                self.off = 0

            def f32(self, shape):
                n = int(np.prod(shape[1:]))
                a = self.off
                self.off += (n + 7) // 8 * 8
                assert self.off * 4 <= SCR_BYTES, (self.off * 4, SCR_BYTES)
                v = SCR[0:shape[0], a:a + n]
                return _reshape(v, shape)

            def bf16(self, shape):
                n = int(np.prod(shape[1:]))
                nw = (n + 1) // 2
                a = self.off
                self.off += (nw + 7) // 8 * 8
                assert self.off * 4 <= SCR_BYTES, (self.off * 4, SCR_BYTES)
                v = SCR[0:shape[0], a:a + nw].bitcast(BF16)[:, 0:n]
                return _reshape(v, shape)

        def _reshape(v, shape):
            if len(shape) == 2:
                return v
            if len(shape) == 3:
                return v.rearrange("p (a b) -> p a b", a=shape[1])
            if len(shape) == 4:
                return v.rearrange("p (a b c) -> p a b c", a=shape[1], b=shape[2])
            raise ValueError

        scr = Scratch()

        def bc(ap2d, mid):
            return ap2d.unsqueeze(1).broadcast_to([ap2d.shape[0], mid, ap2d.shape[1]])

        def bcl(ap2d, last):
            return ap2d.unsqueeze(2).broadcast_to([ap2d.shape[0], ap2d.shape[1], last])

        bH = [Buf(f"H{c}") for c in range(NCHUNK)]
        bXN = [Buf(f"XN{c}") for c in range(NCHUNK)]
        bRING = [Buf(f"ring{i}") for i in range(NRING)]
        bSTG = [Buf(f"stg{i}") for i in range(NSTG)]
        bC = Buf("consts")
        bSQ = [Buf("sq0"), Buf("sq1")]
        bRS = [Buf("rs0"), Buf("rs1")]
        bLT = Buf("ltmp")
        stg_sem = [S.dma_slot("dstg") for _ in range(NSTG)]
        c_sem = S.dma_slot("dconst")
        in_sem = [S.dma_slot("din") for _ in range(4)]
        out_sem = [S.dma_slot("dout") for _ in range(6)]
        dbg_sem = S.dma_slot("ddbg")

        def chunks_of(t0, t1):
            return list(range(t0 // N, (t1 - 1) // N + 1))

        def Hb(t0, t1):
            return [bH[c] for c in chunks_of(t0, t1)]

        def XNb(t0, t1):
            return [bXN[c] for c in chunks_of(t0, t1)]

        def consts():
            S.op("pool", lambda e: e.memset(ident_f[:], 0.0), writes=[bC])
            S.op("pool", lambda e: e.affine_select(out=ident_f[:], in_=ident_f[:], pattern=[[-1, 128]],
                                                   compare_op=ALU.not_equal, fill=1.0, base=0,
                                                   channel_multiplier=1), writes=[bC])
            S.op("pool", lambda e: e.tensor_copy(out=ident_b[:], in_=ident_f[:]), reads=[bC], writes=[bC])
            S.op("pool", lambda e: e.memset(ones_f[:], 1.0), writes=[bC])
            S.op("pool", lambda e: e.memset(ones_b[:], 1.0), writes=[bC])
            tmp = SQ[0][:, 0:128]
            S.op("pool", lambda e: e.memset(tmp, 1.0), writes=[bSQ[0]])
            S.op("pool", lambda e: e.affine_select(out=tmp, in_=tmp, pattern=[[1, 128]], compare_op=ALU.is_ge,
                                                   fill=0.0, base=0, channel_multiplier=-1), writes=[bSQ[0]])
            S.op("pool", lambda e: e.tensor_copy(out=mask01[:], in_=tmp), reads=[bSQ[0]], writes=[bC])
            S.op("pool", lambda e: e.tensor_scalar(out=tmp, in0=tmp, scalar1=-1.0, scalar2=30000.0, op0=ALU.add,
                                                   op1=ALU.mult), reads=[bSQ[0]], writes=[bSQ[0]])
            S.op("pool", lambda e: e.tensor_copy(out=maskneg[:], in_=tmp), reads=[bSQ[0]], writes=[bC])
            scr.reset()
            bt = Buf("selt", S.barrier())
            self_f = scr.f32([128, 16, 128])
            S.op("pool", lambda e: e.memset(self_f, 0.0), writes=[bt])
            S.op("pool", lambda e: e.affine_select(out=self_f, in_=self_f, pattern=[[-1, 16], [0, 128]],
                                                   compare_op=ALU.not_equal, fill=1.0, base=0,
                                                   channel_multiplier=1), writes=[bt])
            S.op("pool", lambda e: e.tensor_copy(out=SEL[:], in_=self_f), reads=[bt], writes=[bC])
            gsrc = [dr["a_norm"][0], dr["ffn_norm"][0], dr["ffn_norm"][1], dr["kv_norm"], dr["b_norm"][0],
                    dr["final_norm"]]
            for i, g in enumerate(gsrc):
                S.dma("sp", lambda e, i=i, g=g: e.dma_start(out=G[:, i, :], in_=g.rearrange("(k p) -> p k", p=128),
                                                            allow_slow_non_contiguous=True), c_sem, writes=[bC])
            S.dma("sp", lambda e: e.dma_start(out=CW[:], in_=dr["a_conv"][0].rearrange("j (k p) -> p j k", p=128),
                                              allow_slow_non_contiguous=True), c_sem, writes=[bC])
            for l in range(2):
                S.dma("sp", lambda e, l=l: e.dma_start(out=WR32[:, l, :, 0:4],
                                                       in_=dr["router_group_w"][l].rearrange("(k p) n -> p k n", p=128)),
                      c_sem, writes=[bC])
                S.dma("sp", lambda e, l=l: e.dma_start(out=WR32[:, l, :, 4:20],
                                                       in_=dr["router_expert_w"][l].rearrange("(k p) n -> p k n", p=128)),
                      c_sem, writes=[bC])
                S.dma("sp", lambda e, l=l: e.dma_start(out=RB[:, l, 0:4],
                                                       in_=dr["router_group_b"][l:l + 1, :].broadcast_to([128, 4])),
                      c_sem, writes=[bC])
                S.dma("sp", lambda e, l=l: e.dma_start(out=RB[:, l, 4:20],
                                                       in_=dr["router_expert_b"][l:l + 1, :].broadcast_to([128, 16])),
                      c_sem, writes=[bC])
            S.dma("sp", lambda e: e.dma_start(out=GS[:], in_=dr["b_subln"][0:1, :].broadcast_to([128, 128])),
                  c_sem, writes=[bC])
            S.dma("sp", lambda e: e.dma_start(out=GSC[:], in_=dr["b_subln"][0].rearrange("(p o) -> p o", o=1)),
                  c_sem, writes=[bC])
            lam = scr.f32([128, 4, 64])
            bl = Buf("lam", S.barrier())
            S.dma("sp", lambda e: e.dma_start(out=lam, in_=dr["b_lambda"][0:1, :, :].broadcast_to([128, 4, 64])),
                  c_sem, writes=[bl])
            for l in range(2):
                S.op("pool", lambda e, l=l: e.tensor_tensor(out=WRS[:, l, :, :], in0=WR32[:, l, :, :],
                                                            in1=bcl(G[:, G_F0 + l, :], 20), op=ALU.mult),
                     reads=[bC], writes=[bC])
            S.op("pool", lambda e: e.tensor_scalar(out=GS[:], in0=GS[:], scalar1=1.0 - LAMBDA_INIT, scalar2=None,
                                                   op0=ALU.mult), reads=[bC], writes=[bC])
            S.op("pool", lambda e: e.tensor_scalar(out=GSC[:], in0=GSC[:], scalar1=1.0 - LAMBDA_INIT, scalar2=None,
                                                   op0=ALU.mult), reads=[bC], writes=[bC])
            p01 = scr.f32([128, 64])
            p23 = scr.f32([128, 64])
            S.op("dve", lambda e: e.tensor_tensor(out=p01, in0=lam[:, 0, :], in1=lam[:, 1, :], op=ALU.mult),
                 reads=[bl], writes=[bt])
            S.op("dve", lambda e: e.reduce_sum(out=LTMP[:, 0:1], in_=p01, axis=AX.X), reads=[bt], writes=[bLT])
            S.op("dve", lambda e: e.tensor_tensor(out=p23, in0=lam[:, 2, :], in1=lam[:, 3, :], op=ALU.mult),
                 reads=[bl], writes=[bt])
            S.op("dve", lambda e: e.reduce_sum(out=LTMP[:, 1:2], in_=p23, axis=AX.X), reads=[bt], writes=[bLT])
            S.op("act", lambda e: e.activation(out=LTMP[:, 2:4], in_=LTMP[:, 0:2], func=AF.Exp), reads=[bLT],
                 writes=[bLT])
            S.op("dve", lambda e: e.tensor_tensor(out=LTMP[:, 4:5], in0=LTMP[:, 3:4], in1=LTMP[:, 2:3],
                                                  op=ALU.subtract), reads=[bLT], writes=[bLT])
            S.op("dve", lambda e: e.tensor_scalar(out=NEGLAM[:], in0=LTMP[:, 4:5], scalar1=-LAMBDA_INIT, scalar2=None,
                                                  op0=ALU.add), reads=[bLT], writes=[bC])

        units = []
        state = dict(dma_next=0, cast_next=0)

        def unitA(w2d, col0, gain):
            units.append((w2d[:, col0:col0 + 256].rearrange("(k p) n -> p k n", p=128), gain, 8, 256))
            return len(units) - 1

        def unitB(w2d, row0, col0):
            units.append((w2d[row0:row0 + 512, col0:col0 + 512].rearrange("(k p) n -> p k n", p=128), None, 4, 512))
            return len(units) - 1

        def unitC(w2d, row0):
            units.append((w2d[row0:row0 + 256, :].rearrange("(k p) n -> p k n", p=128), None, 2, 1024))
            return len(units) - 1

        def ring_view(u):
            _, _, nk, ncol = units[u]
            return RING[:, u % NRING, :].rearrange("p (k n) -> p k n", k=nk)

        def rb(u):
            return bRING[u % NRING]

        def rec_dma(u):
            src, gain, nk, ncol = units[u]
            s = u % NSTG
            dst = STG[:, s, :].rearrange("p (k n) -> p k n", k=nk)
            S.dma("sp", lambda e: e.dma_start(out=dst, in_=src), stg_sem[s], writes=[bSTG[s]])

        def rec_cast(u):
            src, gain, nk, ncol = units[u]
            s = u % NSTG
            stv = STG[:, s, :].rearrange("p (k n) -> p k n", k=nk)
            dst = ring_view(u)
            if gain is None:
                S.op("pool", lambda e: e.tensor_copy(out=dst, in_=stv), reads=[bSTG[s]], writes=[rb(u)])
            else:
                S.op("pool", lambda e: e.tensor_tensor(out=dst, in0=stv, in1=bcl(G[:, gain, :], ncol), op=ALU.mult),
                     reads=[bSTG[s], bC], writes=[rb(u)])

        def ensure_cast(u):
            u = min(u, len(units) - 1)
            while state["cast_next"] <= u:
                j = state["cast_next"]
                while state["dma_next"] <= min(j + NSTG - 1, len(units) - 1):
                    rec_dma(state["dma_next"])
                    state["dma_next"] += 1
                rec_cast(j)
                state["cast_next"] += 1
                while state["dma_next"] <= min(j + NSTG, len(units) - 1):
                    rec_dma(state["dma_next"])
                    state["dma_next"] += 1

        def mm_group(out_ap, pairs, reads, writes):
            def fn(e):
                n = len(pairs)
                ins = None
                for i, (l, r) in enumerate(pairs):
                    ins = e.matmul(out_ap, lhsT=l, rhs=r, start=(i == 0), stop=(i == n - 1))
                return ins
            return S.op("pe", fn, reads=reads, writes=writes)

        def norm_stats(c, ri, rsx=None):
            c0 = c * N
            RS_, bRS_ = (RS[ri], bRS[ri]) if rsx is None else rsx
            for k in range(KC):
                q = k % 2
                S.op("act", lambda e, k=k, q=q: e.activation(out=SQB[q], in_=H[:, k, c0:c0 + N], func=AF.Square),
                     reads=[bH[c]], writes=[bSQ[q]])
                S.op("pe", lambda e, k=k, q=q: e.matmul(psum[6][:, 0:N], lhsT=ones_b[:], rhs=SQB[q],
                                                        start=(k == 0), stop=(k == KC - 1)),
                     reads=[bSQ[q], bC], writes=[PB[6]])
            S.op("dve", lambda e: e.tensor_scalar(out=RS_[:], in0=psum[6][:, 0:N], scalar1=1.0 / D, scalar2=RMS_EPS,
                                                  op0=ALU.mult, op1=ALU.add), reads=[PB[6]], writes=[bRS_])
            S.op("act", lambda e: e.activation(out=RS_[:], in_=RS_[:], func=AF.Sqrt), reads=[bRS_], writes=[bRS_])
            S.op("dve", lambda e: e.reciprocal(out=RS_[:], in_=RS_[:]), reads=[bRS_], writes=[bRS_])

        def norm_to_xn(c, ri, rsx=None):
            c0 = c * N
            RS_, bRS_ = (RS[ri], bRS[ri]) if rsx is None else rsx
            S.op("dve", lambda e: e.tensor_tensor(out=XN[:, :, c0:c0 + N], in0=H[:, :, c0:c0 + N],
                                                  in1=bc(RS_[:], KC), op=ALU.mult),
                 reads=[bH[c], bRS_], writes=[bXN[c]])

        def dump_dbg():
            t = S.dma("sp", lambda e: e.dma_start(out=dbg_out[:, :, :], in_=H[:]), dbg_sem, reads=bH)
            return t

        def phase_load(s):
            scr.reset()
            bar = S.barrier()
            NXT = 4
            XT = [scr.f32([128, D]) for _ in range(NXT)]
            bXT = [Buf(f"xt{i}", bar) for i in range(NXT)]
            ntile = (L + 127) // 128
            tcount = 0
            for i in range(ntile):
                t0 = i * 128
                nt = min(128, L - t0)
                q = i % NXT
                if i == 0:
                    S.dma("sp", lambda e, q=q: e.dma_start(out=XT[q][0:NMETA, :], in_=dr["meta_tokens"][:, :]),
                          in_sem[q], writes=[bXT[q]])
                    S.dma("sp", lambda e, q=q: e.dma_start(out=XT[q][NMETA:128, :], in_=dr["x"][s, 0:128 - NMETA, :]),
                          in_sem[q], writes=[])
                    bXT[q].w = {in_sem[q][0]: in_sem[q][1]}
                else:
                    S.dma("sp", lambda e, q=q, t0=t0, nt=nt: e.dma_start(out=XT[q][0:nt, :],
                                                                          in_=dr["x"][s, t0 - NMETA:t0 - NMETA + nt, :]),
                          in_sem[q], writes=[bXT[q]])
                for k in range(KC):
                    pb = tcount % 4
                    tcount += 1
                    S.op("pe", lambda e, q=q, k=k, nt=nt, pb=pb: e.transpose(out=psum[pb][:, 0:nt],
                                                                              in_=XT[q][0:nt, k * 128:(k + 1) * 128],
                                                                              identity=ident_f[0:nt, 0:nt]),
                         reads=[bXT[q], bC], writes=[PB[pb]])
                    if k % 2 == 0:
                        S.op("dve", lambda e, k=k, t0=t0, nt=nt, pb=pb: e.tensor_copy(out=H[:, k, t0:t0 + nt],
                                                                                      in_=psum[pb][:, 0:nt]),
                             reads=[PB[pb]], writes=Hb(t0, t0 + nt))
                    else:
                        S.op("act", lambda e, k=k, t0=t0, nt=nt, pb=pb: e.copy(out=H[:, k, t0:t0 + nt],
                                                                               in_=psum[pb][:, 0:nt]),
                             reads=[PB[pb]], writes=Hb(t0, t0 + nt))

        def conv_units():
            w_in = dr["a_w_in"][0]
            w_out = dr["a_w_out"][0]
            ids = []
            for hf in range(2):
                for j in (2 * hf, 2 * hf + 1):
                    ids.append(("b", j, unitA(w_in, 256 * j, G_A)))
                    ids.append(("c", j, unitA(w_in, 1024 + 256 * j, G_A)))
                    ids.append(("u", j, unitA(w_in, 2048 + 256 * j, G_A)))
                ids.append(("o", hf, unitB(w_out, 512 * hf, 0)))
                ids.append(("o2", hf, unitB(w_out, 512 * hf, 512)))
            return ids

        def phase_conv(ids):
            scr.reset()
            bar = S.barrier()
            Z = scr.f32([128, L + 2])
            CS = [scr.f32([128, N]) for _ in range(2)]
            CV = [scr.f32([128, N]) for _ in range(2)]
            BZ = scr.bf16([128, 4, L])
            bZ = [Buf(f"z{c}", bar) for c in range(NCHUNK)]
            bZ0 = Buf("zpad", bar)
            bCS = [Buf("cs0", bar), Buf("cs1", bar)]
            bCV = [Buf("cv0", bar), Buf("cv1", bar)]
            bBZ = [Buf(f"bz{c}", bar) for c in range(NCHUNK)]
            um = {(n, j): u for n, j, u in ids}
            for c in range(NCHUNK):
                norm_stats(c, c % 2)
                norm_to_xn(c, c % 2)
            S.op("pool", lambda e: e.memset(Z[:, 0:2], 0.0), writes=[bZ0])
            it = 0
            for hf in range(2):
                for j in (2 * hf, 2 * hf + 1):
                    ub, uc, uu = um[("b", j)], um[("c", j)], um[("u", j)]
                    ensure_cast(uu + 2)
                    vb, vc, vu = ring_view(ub), ring_view(uc), ring_view(uu)
                    for mm in range(2):
                        m = 2 * j + mm
                        ml = m - 4 * hf
                        for c in range(NCHUNK):
                            c0 = c * N
                            q = it % 2
                            it += 1
                            pb_, pc_, pu_ = q, 2 + q, 4 + q
                            cols = slice(mm * 128, (mm + 1) * 128)
                            mm_group(psum[pc_][:, 0:N], [(vc[:, k, cols], XN[:, k, c0:c0 + N]) for k in range(KC)],
                                     reads=[rb(uc), bXN[c]], writes=[PB[pc_]])
                            mm_group(psum[pu_][:, 0:N], [(vu[:, k, cols], XN[:, k, c0:c0 + N]) for k in range(KC)],
                                     reads=[rb(uu), bXN[c]], writes=[PB[pu_]])
                            mm_group(psum[pb_][:, 0:N], [(vb[:, k, cols], XN[:, k, c0:c0 + N]) for k in range(KC)],
                                     reads=[rb(ub), bXN[c]], writes=[PB[pb_]])
                            S.op("act", lambda e, q=q, pc_=pc_: e.copy(out=CS[q][:], in_=psum[pc_][:, 0:N]),
                                 reads=[PB[pc_]], writes=[bCS[q]])
                            S.op("dve", lambda e, q=q, pu_=pu_, c0=c0: e.tensor_tensor(out=Z[:, 2 + c0:2 + c0 + N],
                                                                                       in0=psum[pu_][:, 0:N], in1=CS[q][:],
                                                                                       op=ALU.mult),
                                 reads=[PB[pu_], bCS[q]], writes=[bZ[c]])
                            zr = [bZ[c], bZ[c - 1] if c > 0 else bZ0]
                            S.op("act", lambda e, q=q, c0=c0, m=m: e.activation(
                                out=CV[q][:], in_=Z[:, 2 + c0:2 + c0 + N], func=AF.Copy, scale=CW[:, 2, m:m + 1]),
                                reads=zr + [bC], writes=[bCV[q]])
                            S.op("dve", lambda e, q=q, c0=c0, m=m: e.scalar_tensor_tensor(
                                out=CV[q][:], in0=Z[:, 1 + c0:1 + c0 + N], scalar=CW[:, 1, m:m + 1], in1=CV[q][:],
                                op0=ALU.mult, op1=ALU.add), reads=zr + [bC], writes=[bCV[q]])
                            S.op("dve", lambda e, q=q, c0=c0, m=m: e.scalar_tensor_tensor(
                                out=CV[q][:], in0=Z[:, c0:c0 + N], scalar=CW[:, 0, m:m + 1], in1=CV[q][:],
                                op0=ALU.mult, op1=ALU.add), reads=zr + [bC], writes=[bCV[q]])
                            S.op("dve", lambda e, q=q, pb_=pb_, c0=c0, ml=ml: e.tensor_tensor(
                                out=BZ[:, ml, c0:c0 + N], in0=psum[pb_][:, 0:N], in1=CV[q][:], op=ALU.mult),
                                reads=[PB[pb_], bCV[q]], writes=[bBZ[c]])
                uo = [um[("o", hf)], um[("o2", hf)]]
                ensure_cast(uo[1] + 2)
                for o in range(KC):
                    vo = ring_view(uo[o // 4])
                    cols = slice((o % 4) * 128, (o % 4 + 1) * 128)
                    for c in range(NCHUNK):
                        c0 = c * N
                        pd = (6, 7, 0, 1)[it % 4]
                        it += 1
                        mm_group(psum[pd][:, 0:N], [(vo[:, k, cols], BZ[:, k, c0:c0 + N]) for k in range(4)],
                                 reads=[rb(uo[o // 4]), bBZ[c]], writes=[PB[pd]])
                        S.op("dve", lambda e, o=o, c0=c0, pd=pd: e.tensor_tensor(out=H[:, o, c0:c0 + N],
                                                                                 in0=psum[pd][:, 0:N],
                                                                                 in1=H[:, o, c0:c0 + N], op=ALU.add),
                             reads=[PB[pd]], writes=[bH[c]])

        def moe_units(layer):
            ids = []
            for ex in range(NEXP):
                wg = dr["expert_w_gate"][layer, ex]
                wu = dr["expert_w_up"][layer, ex]
                wd = dr["expert_w_down"][layer, ex]
                gi = G_F0 + layer
                u0 = unitA(wg, 0, gi)
                unitA(wg, 256, gi)
                unitA(wu, 0, gi)
                unitA(wu, 256, gi)
                unitB(wd, 0, 0)
                unitB(wd, 0, 512)
                ids.append(u0)
            return ids

        def phase_moe(layer, ids):
            scr.reset()
            bar = S.barrier()
            HE = [scr.bf16([128, 4, N]) for _ in range(2)]
            SIL = [scr.f32([128, N]) for _ in range(2)]
            CB = [scr.f32([128, N]) for _ in range(2)]
            CT = scr.bf16([128, L])
            XR = [scr.f32([128, N]) for _ in range(2)]
            LGT = [scr.f32([20, N]) for _ in range(2)]
            NT = 18
            LG = scr.f32([128, NT, 20])
            LEM = scr.f32([128, NT, 16])
            OH1 = scr.f32([128, NT, 16])
            LEM2 = scr.f32([128, NT, 16])
            OH2 = scr.f32([128, NT, 16])
            CMB = scr.f32([128, NT, 16])
            OHG = scr.f32([128, NT, 4])
            EG = scr.f32([128, NT, 4])
            PEN = scr.f32([128, NT, 4])
            SC = scr.f32([128, 8, NT])
            bHE = [Buf("he0", bar), Buf("he1", bar)]
            bSIL = [Buf("sil0", bar), Buf("sil1", bar)]
            bCB = [Buf("cb0", bar), Buf("cb1", bar)]
            bCT = Buf("ct", bar)
            bXR = [Buf("xr0", bar), Buf("xr1", bar)]
            bLGT = [Buf("lgt0", bar), Buf("lgt1", bar)]
            bLG = Buf("lg", bar)
            bR = Buf("rt", bar)

            def topk(ta, tb):
                nT = tb - ta
                lg4 = LG[:, ta:tb, 0:4]
                le16 = LG[:, ta:tb, 4:20]
                MG, SE, PSEL, V1, V2, DD, W1, W2 = [SC[:, i, ta:tb] for i in range(8)]
                OHG_, EG_, PEN_ = OHG[:, ta:tb, :], EG[:, ta:tb, :], PEN[:, ta:tb, :]
                LEM_, OH1_, LEM2_, OH2_, CMB_ = (LEM[:, ta:tb, :], OH1[:, ta:tb, :], LEM2[:, ta:tb, :], OH2[:, ta:tb, :],
                                                 CMB[:, ta:tb, :])

                def dv(fn):
                    S.op("dve", fn, reads=[bR, bLG], writes=[bR])

                dv(lambda e: e.tensor_reduce(out=MG, in_=lg4, axis=AX.X, op=ALU.max))
                dv(lambda e: e.tensor_tensor(out=OHG_, in0=lg4, in1=bcl(MG, 4), op=ALU.is_equal))
                dv(lambda e: e.tensor_tensor(out=EG_, in0=lg4, in1=bcl(MG, 4), op=ALU.subtract))
                S.op("act", lambda e: e.activation(out=EG_, in_=EG_, func=AF.Exp), reads=[bR], writes=[bR])
                dv(lambda e: e.tensor_reduce(out=SE, in_=EG_, axis=AX.X, op=ALU.add))
                dv(lambda e: e.reciprocal(out=PSEL, in_=SE))
                dv(lambda e: e.tensor_scalar(out=PEN_, in0=OHG_, scalar1=-1.0, scalar2=BIG, op0=ALU.add, op1=ALU.mult))
                dv(lambda e: e.tensor_tensor(out=LEM_.rearrange("p t (g j) -> p t g j", g=4),
                                             in0=le16.rearrange("p t (g j) -> p t g j", g=4),
                                             in1=PEN_.unsqueeze(3).broadcast_to([128, nT, 4, 4]), op=ALU.add))
                dv(lambda e: e.tensor_reduce(out=V1, in_=LEM_, axis=AX.X, op=ALU.max))
                dv(lambda e: e.tensor_tensor(out=OH1_, in0=LEM_, in1=bcl(V1, 16), op=ALU.is_equal))
                dv(lambda e: e.scalar_tensor_tensor(out=LEM2_, in0=OH1_, scalar=-BIG, in1=LEM_, op0=ALU.mult, op1=ALU.add))
                dv(lambda e: e.tensor_reduce(out=V2, in_=LEM2_, axis=AX.X, op=ALU.max))
                dv(lambda e: e.tensor_tensor(out=OH2_, in0=LEM2_, in1=bcl(V2, 16), op=ALU.is_equal))
                dv(lambda e: e.tensor_tensor(out=DD, in0=V2, in1=V1, op=ALU.subtract))
                S.op("act", lambda e: e.activation(out=DD, in_=DD, func=AF.Exp), reads=[bR], writes=[bR])
                dv(lambda e: e.tensor_scalar(out=W1, in0=DD, scalar1=1.0, scalar2=None, op0=ALU.add))
                dv(lambda e: e.reciprocal(out=W1, in_=W1))
                dv(lambda e: e.tensor_tensor(out=W2, in0=DD, in1=W1, op=ALU.mult))
                dv(lambda e: e.tensor_tensor(out=W1, in0=W1, in1=PSEL, op=ALU.mult))
                dv(lambda e: e.tensor_tensor(out=W2, in0=W2, in1=PSEL, op=ALU.mult))
                dv(lambda e: e.tensor_tensor(out=OH1_, in0=OH1_, in1=bcl(W1, 16), op=ALU.mult))
                dv(lambda e: e.tensor_tensor(out=OH2_, in0=OH2_, in1=bcl(W2, 16), op=ALU.mult))
                dv(lambda e: e.tensor_tensor(out=CMB_, in0=OH1_, in1=OH2_, op=ALU.add))
                for ti in range(ta, tb):
                    t0, nt = tiles[ti]
                    pb = ti % 2
                    S.op("pe", lambda e, ti=ti, nt=nt, pb=pb: e.transpose(out=psum[pb][0:16, 0:nt], in_=CMB[0:nt, ti, :],
                                                                          identity=ident_f[0:nt, 0:nt]),
                         reads=[bR, bC], writes=[PB[pb]])
                    S.op("act", lambda e, t0=t0, nt=nt, pb=pb: e.copy(out=CT[0:16, t0:t0 + nt], in_=psum[pb][0:16, 0:nt]),
                         reads=[PB[pb]], writes=[bCT])

            S.op("pool", lambda e: e.memset(LG[:], 0.0), writes=[bLG])
            S.op("pool", lambda e: e.memset(CT[:], 0.0), writes=[bCT])
            tiles = []
            xi = 0
            rsx = [(RS[0], bRS[0]), (RS[1], bRS[1]), (SIL[0], bSIL[0]), (SIL[1], bSIL[1]), (CB[0], bCB[0]),
                   (CB[1], bCB[1])]
            for c in range(NCHUNK):
                norm_stats(c, None, rsx[c])
                norm_to_xn(c, None, rsx[c])
            for c in range(NCHUNK):
                c0 = c * N
                RSc, bRSc = rsx[c]
                for k in range(KC):
                    xq = xi % 2
                    xi += 1
                    S.op("dve" if k % 2 == 0 else "pool", lambda e, k=k, xq=xq, c0=c0, RSc=RSc: e.tensor_tensor(
                        out=XR[xq][:], in0=H[:, k, c0:c0 + N], in1=RSc[:], op=ALU.mult),
                        reads=[bH[c], bRSc], writes=[bXR[xq]])
                    S.op("pe", lambda e, k=k, xq=xq: e.matmul(psum[7][0:20, 0:N], lhsT=WRS[:, layer, k, :], rhs=XR[xq][:],
                                                              start=(k == 0), stop=(k == KC - 1)),
                         reads=[bXR[xq], bC], writes=[PB[7]])
                lq = c % 2
                S.op("act", lambda e, lq=lq: e.copy(out=LGT[lq][:], in_=psum[7][0:20, 0:N]), reads=[PB[7]],
                     writes=[bLGT[lq]])
                for (o, nt) in ((0, 128), (128, 128), (256, N - 256)):
                    ti = len(tiles)
                    t0 = c0 + o
                    tiles.append((t0, nt))
                    rbk = 5 if ti % 2 == 0 else 4
                    S.op("pe", lambda e, lq=lq, o=o, nt=nt, rbk=rbk: e.transpose(out=psum[rbk][0:nt, 0:20],
                                                                                 in_=LGT[lq][0:20, o:o + nt],
                                                                                 identity=ident_f[0:20, 0:20]),
                         reads=[bLGT[lq], bC], writes=[PB[rbk]])
                    S.op("dve", lambda e, ti=ti, nt=nt, rbk=rbk: e.tensor_tensor(out=LG[0:nt, ti, :], in0=psum[rbk][0:nt, 0:20],
                                                                                 in1=RB[0:nt, layer, :], op=ALU.add),
                         reads=[PB[rbk], bC], writes=[bLG])
            topk(0, 3 * NCHUNK)
            assert len(tiles) == NT
            it = 0

            def gate_up(ex, c, hq, ug, uu):
                nonlocal it
                c0 = c * N
                S.op("pe", lambda e: e.matmul(psum[0][:, 0:N], lhsT=SEL[:, ex, :], rhs=CT[:, c0:c0 + N],
                                              start=True, stop=True), reads=[bC, bCT], writes=[PB[0]])
                S.op("act", lambda e: e.copy(out=CB[hq][:], in_=psum[0][:, 0:N]), reads=[PB[0]], writes=[bCB[hq]])
                for m in range(4):
                    q = it % 2
                    it += 1
                    pg, pu = 2 + q, 4 + q
                    vg, vu = ring_view(ug[m // 2]), ring_view(uu[m // 2])
                    cols = slice((m % 2) * 128, (m % 2 + 1) * 128)
                    mm_group(psum[pg][:, 0:N], [(vg[:, k, cols], XN[:, k, c0:c0 + N]) for k in range(KC)],
                             reads=[rb(ug[m // 2]), bXN[c]], writes=[PB[pg]])
                    mm_group(psum[pu][:, 0:N], [(vu[:, k, cols], XN[:, k, c0:c0 + N]) for k in range(KC)],
                             reads=[rb(uu[m // 2]), bXN[c]], writes=[PB[pu]])
                    S.op("act", lambda e, q=q, pg=pg: e.activation(out=SIL[q][:], in_=psum[pg][:, 0:N], func=AF.Silu),
                         reads=[PB[pg]], writes=[bSIL[q]])
                    S.op("dve", lambda e, q=q, pu=pu: e.tensor_tensor(out=SIL[q][:], in0=psum[pu][:, 0:N],
                                                                      in1=SIL[q][:], op=ALU.mult),
                         reads=[PB[pu], bSIL[q]], writes=[bSIL[q]])
                    S.op("pool", lambda e, q=q, m=m: e.tensor_tensor(out=HE[hq][:, m, :], in0=SIL[q][:],
                                                                     in1=CB[hq][:], op=ALU.mult),
                         reads=[bSIL[q], bCB[hq]], writes=[bHE[hq]])

            dcnt = [0]

            def down(c, hq, ud):
                c0 = c * N
                for o in range(KC):
                    pd = (1, 6, 7)[dcnt[0] % 3]
                    dcnt[0] += 1
                    vd = ring_view(ud[o // 4])
                    cols = slice((o % 4) * 128, (o % 4 + 1) * 128)
                    mm_group(psum[pd][:, 0:N], [(vd[:, k, cols], HE[hq][:, k, :]) for k in range(4)],
                             reads=[rb(ud[o // 4]), bHE[hq]], writes=[PB[pd]])
                    S.op("dve", lambda e, o=o, pd=pd: e.tensor_tensor(out=H[:, o, c0:c0 + N], in0=psum[pd][:, 0:N],
                                                                      in1=H[:, o, c0:c0 + N], op=ALU.add),
                         reads=[PB[pd]], writes=[bH[c]])

            prev = None
            idx = 0
            for ex in range(NEXP):
                u0 = ids[ex]
                ug, uu, ud = (u0, u0 + 1), (u0 + 2, u0 + 3), (u0 + 4, u0 + 5)
                ensure_cast(u0 + 5)
                for c in range(NCHUNK):
                    ensure_cast(u0 + 6 + c)
                    hq = idx % 2
                    idx += 1
                    gate_up(ex, c, hq, ug, uu)
                    if prev is not None:
                        down(*prev)
                    prev = (c, hq, ud)
            down(*prev)

        def attn_units():
            ids = []
            wq = dr["b_w_q"][0]
            wkv = dr["w_kv"]
            wo = dr["b_w_o"][0]
            for j in range(4):
                uq = unitA(wq, 256 * j, G_B)
                unitA(wkv, 256 * j, G_KV)
                unitA(wkv, 1024 + 256 * j, G_KV)
                unitC(wo, 256 * j)
                ids.append(uq)
            return ids

        def phase_attn(ids):
            scr.reset()
            bar = S.barrier()
            QW = 384
            NPT = 5
            SKEW = 2
            QT = [scr.bf16([128, L]) for _ in range(2)]
            KT = scr.bf16([128, L])
            VA = scr.bf16([128, 17, 128])
            OT = scr.bf16([128, L])
            PT = [scr.bf16([128, QW]) for _ in range(NPT)]
            R0 = scr.f32([128, QW])
            R1 = scr.f32([128, QW])
            OA = scr.f32([128, QW])
            OB2 = scr.f32([128, QW])
            SQb = scr.bf16([128, QW])
            bQT, bKT, bVA = Buf("qt", bar), Buf("kt", bar), Buf("va", bar)
            bOT = [Buf(f"ot{c}", bar) for c in range(NCHUNK)]
            bPT = [Buf(f"pt{i}", bar) for i in range(NPT)]
            bE = Buf("epi", bar)
            bHT = [Buf("ht0", bar), Buf("ht1", bar)]
            SB_ = (0, 1, 2)
            OBK = (3, 5)
            SBK = (4, 6)
            MB_ = (7, 5, 6)

            for c in range(NCHUNK):
                norm_stats(c, c % 2)
                norm_to_xn(c, c % 2)
            S.op("pool", lambda e: e.memset(QT[0][64:128, :], 0.0), writes=[bQT])
            S.op("pool", lambda e: e.memset(QT[1][0:64, :], 0.0), writes=[bQT])
            qchunks = []
            q = 0
            while q < L:
                qchunks.append((q, min(QW, L - q)))
                q += QW
            cnt = dict(s=0, pt=0, mb=0, ep=0)
            deferred = []
            pending_tail = []
            DEFER = 8

            def mbank():
                b = MB_[cnt["mb"] % 3]
                cnt["mb"] += 1
                return b

            for j in range(4):
                uq, uk, uv, uo = ids[j], ids[j] + 1, ids[j] + 2, ids[j] + 3
                ensure_cast(uo + 2)
                vq, vk, vv, vo = ring_view(uq), ring_view(uk), ring_view(uv), ring_view(uo)
                for hh in range(2):
                    cols = slice(hh * 128, (hh + 1) * 128)
                    for c in range(NCHUNK):
                        c0 = c * N
                        pq = mbank()
                        mm_group(psum[pq][:, 0:N], [(vq[:, k, cols], XN[:, k, c0:c0 + N]) for k in range(KC)],
                                 reads=[rb(uq), bXN[c]], writes=[PB[pq]])
                        S.op("act", lambda e, c0=c0, pq=pq: e.activation(out=QT[0][0:64, c0:c0 + N], in_=psum[pq][0:64, 0:N],
                                                                         func=AF.Copy, scale=0.125),
                             reads=[PB[pq]], writes=[bQT])
                        S.op("act", lambda e, c0=c0, pq=pq: e.activation(out=QT[1][64:128, c0:c0 + N],
                                                                         in_=psum[pq][64:128, 0:N], func=AF.Copy, scale=0.125),
                             reads=[PB[pq]], writes=[bQT])
                        pq = mbank()
                        mm_group(psum[pq][:, 0:N], [(vk[:, k, cols], XN[:, k, c0:c0 + N]) for k in range(KC)],
                                 reads=[rb(uk), bXN[c]], writes=[PB[pq]])
                        S.op("dve", lambda e, c0=c0, pq=pq: e.tensor_copy(out=KT[:, c0:c0 + N], in_=psum[pq][:, 0:N]),
                             reads=[PB[pq]], writes=[bKT])
                    for t in range(17):
                        t0 = t * 128
                        nt = min(128, L - t0)
                        pq = mbank()
                        mm_group(psum[pq][0:nt, 0:128], [(XN[:, k, t0:t0 + nt], vv[:, k, cols]) for k in range(KC)],
                                 reads=[rb(uv)] + XNb(t0, t0 + nt), writes=[PB[pq]])
                        if t % 2 == 0:
                            S.op("dve", lambda e, t=t, nt=nt, pq=pq: e.tensor_copy(out=VA[0:nt, t, 0:128],
                                                                                   in_=psum[pq][0:nt, 0:128]),
                                 reads=[PB[pq]], writes=[bVA])
                        else:
                            S.op("act", lambda e, t=t, nt=nt, pq=pq: e.copy(out=VA[0:nt, t, 0:128],
                                                                            in_=psum[pq][0:nt, 0:128]),
                                 reads=[PB[pq]], writes=[bVA])

                    while pending_tail:
                        pending_tail.pop(0)()
                    def stageA(step):
                        (q0, nq, cc, kt, last, kt_max) = step
                        q1 = q0 + nq
                        rows = slice(cc * 64, (cc + 1) * 64)
                        k0 = kt * 128
                        nk = min(128, L - k0)
                        qlo = max(q0, k0)
                        width = q1 - qlo
                        sb_ = SB_[cnt["s"] % 3]
                        cnt["s"] += 1
                        pi = cnt["pt"] % NPT
                        cnt["pt"] += 1
                        diag = k0 >= q0
                        dw = min(128, width)

                        def qk(e):
                            ins = e.matmul(psum[sb_][0:nk, 0:width], lhsT=KT[:, k0:k0 + nk], rhs=QT[cc][:, qlo:q1],
                                           start=True, stop=not diag)
                            if diag:
                                ins = e.matmul(psum[sb_][0:nk, 0:dw], lhsT=ident_b[0:nk, 0:nk], rhs=maskneg[0:nk, 0:dw],
                                               start=False, stop=True)
                            return ins
                        S.op("pe", qk, reads=[bKT, bQT, bC], writes=[PB[sb_]])
                        S.op("act", lambda e: e.activation(out=PT[pi][0:nk, 0:width], in_=psum[sb_][0:nk, 0:width],
                                                           func=AF.Exp), reads=[PB[sb_]], writes=[bPT[pi]])
                        return (pi, nk, qlo)

                    def stageB(step, a):
                        (q0, nq, cc, kt, last, kt_max) = step
                        (pi, nk, qlo) = a
                        off = qlo - q0
                        width = q0 + nq - qlo

                        def pv(e):
                            e.matmul(psum[OBK[cc]][:, off:nq], lhsT=VA[0:nk, kt, :], rhs=PT[pi][0:nk, 0:width],
                                     start=(kt == 0), stop=(kt == kt_max))
                            return e.matmul(psum[SBK[cc]][:, off:nq], lhsT=ones_b[0:nk, :], rhs=PT[pi][0:nk, 0:width],
                                            start=(kt == 0), stop=(kt == kt_max))
                        S.op("pe", pv, reads=[bPT[pi], bVA, bC], writes=[PB[OBK[cc]], PB[SBK[cc]]])
                        if last:
                            epilogue(q0, nq)

                    def epilogue(q0, nq):
                        q1 = q0 + nq
                        rd, wr = [bE, bC], [bE]
                        o0, s0 = psum[OBK[0]][:, 0:nq], psum[SBK[0]][:, 0:nq]
                        o1, s1 = psum[OBK[1]][:, 0:nq], psum[SBK[1]][:, 0:nq]
                        S.op("dve", lambda e: e.tensor_copy(out=R0[:, 0:nq], in_=s0), reads=rd + [PB[SBK[0]]], writes=wr)
                        S.op("dve", lambda e: e.tensor_copy(out=OA[:, 0:nq], in_=o0), reads=rd + [PB[OBK[0]]], writes=wr)
                        S.op("dve", lambda e: e.tensor_copy(out=R1[:, 0:nq], in_=s1), reads=rd + [PB[SBK[1]]], writes=wr)
                        S.op("dve", lambda e: e.tensor_copy(out=OB2[:, 0:nq], in_=o1), reads=rd + [PB[OBK[1]]], writes=wr)
                        S.op("dve", lambda e: e.reciprocal(out=R0[:, 0:nq], in_=R0[:, 0:nq]), reads=rd, writes=wr)
                        S.op("dve", lambda e: e.reciprocal(out=R1[:, 0:nq], in_=R1[:, 0:nq]), reads=rd, writes=wr)
                        S.op("dve", lambda e: e.tensor_tensor(out=OA[:, 0:nq], in0=OA[:, 0:nq], in1=R0[:, 0:nq], op=ALU.mult),
                             reads=rd, writes=wr)
                        S.op("dve", lambda e: e.scalar_tensor_tensor(out=OB2[:, 0:nq], in0=OB2[:, 0:nq], scalar=NEGLAM[:, 0:1],
                                                                     in1=R1[:, 0:nq], op0=ALU.mult, op1=ALU.mult),
                             reads=rd, writes=wr)
                        S.op("pool", lambda e: e.tensor_tensor(out=OT[:, q0:q1], in0=OA[:, 0:nq], in1=OB2[:, 0:nq], op=ALU.add),
                             reads=rd, writes=[bE] + [bOT[cx] for cx in chunks_of(q0, q1)])

                    steps = []
                    for (q0, nq) in qchunks:
                        kt_max = (q0 + nq - 1) // 128
                        for cc in range(2):
                            for kt in range(kt_max + 1):
                                steps.append((q0, nq, cc, kt, cc == 1 and kt == kt_max, kt_max))
                    def tick():
                        for d in deferred:
                            d[0] -= 1
                        while deferred and deferred[0][0] <= 0:
                            deferred.pop(0)[1]()

                    pend = []
                    for st_ in steps:
                        pend.append((st_, stageA(st_)))
                        if len(pend) > SKEW:
                            s0, a0 = pend.pop(0)
                            stageB(s0, a0)
                        tick()
                    while pend:
                        s0, a0 = pend.pop(0)
                        stageB(s0, a0)
                        tick()

                    def head_tail(hh=hh, vo=vo, uo=uo):
                        while deferred:
                            deferred.pop(0)[1]()
                        sq_t = [SQb[:, 0:N], OA[:, 0:N // 2].bitcast(BF16)]
                        rs_t = [R0[:, 0:N], R1[:, 0:N]]
                        for c in range(NCHUNK):
                            c0 = c * N
                            par = c % 2
                            sqv, rsv, bt = sq_t[par], rs_t[par], bHT[par]
                            S.op("pool", lambda e, c0=c0, sqv=sqv: e.tensor_tensor(out=sqv, in0=OT[:, c0:c0 + N],
                                                                                  in1=OT[:, c0:c0 + N], op=ALU.mult),
                                 reads=[bOT[c], bE], writes=[bt])
                            pq = mbank()
                            S.op("pe", lambda e, pq=pq, sqv=sqv: e.matmul(psum[pq][:, 0:N], lhsT=ones_b[:], rhs=sqv, start=True,
                                                                          stop=True), reads=[bt, bC, bE], writes=[PB[pq]])
                            S.op("dve", lambda e, pq=pq, rsv=rsv: e.tensor_scalar(out=rsv, in0=psum[pq][:, 0:N], scalar1=1.0 / 128,
                                                                                  scalar2=SUBLN_EPS, op0=ALU.mult, op1=ALU.add),
                                 reads=[PB[pq], bE], writes=[bt])
                            S.op("act", lambda e, rsv=rsv: e.activation(out=rsv, in_=rsv, func=AF.Ln), reads=[bt, bE], writes=[bt])
                            S.op("act", lambda e, rsv=rsv: e.activation(out=rsv, in_=rsv, func=AF.Exp, scale=-0.5),
                                 reads=[bt, bE], writes=[bt])
                            S.op("dve", lambda e, c0=c0, rsv=rsv: e.scalar_tensor_tensor(
                                out=OT[:, c0:c0 + N], in0=OT[:, c0:c0 + N], scalar=GSC[:, 0:1], in1=rsv, op0=ALU.mult,
                                op1=ALU.mult), reads=[bt, bE, bC, bOT[c]], writes=[bt, bOT[c]])
                        for o in range(KC):
                            for c in range(NCHUNK):
                                c0 = c * N
                                pq = mbank()
                                S.op("pe", lambda e, o=o, c0=c0, pq=pq: e.matmul(
                                    psum[pq][:, 0:N], lhsT=vo[:, hh, o * 128:(o + 1) * 128], rhs=OT[:, c0:c0 + N],
                                    start=True, stop=True), reads=[rb(uo), bOT[c]], writes=[PB[pq]])
                                S.op("dve", lambda e, o=o, c0=c0, pq=pq: e.tensor_tensor(out=H[:, o, c0:c0 + N],
                                                                                         in0=psum[pq][:, 0:N],
                                                                                         in1=H[:, o, c0:c0 + N], op=ALU.add),
                                     reads=[PB[pq]], writes=[bH[c]])
                    pending_tail.append(head_tail)
            while pending_tail:
                pending_tail.pop(0)()

        def phase_final(s):
            scr.reset()
            bar = S.barrier()
            Y = [scr.f32([128, N]) for _ in range(3)]
            OUTT = [scr.f32([128, D]) for _ in range(6)]
            bY = [Buf("y0", bar), Buf("y1", bar), Buf("y2", bar)]
            bOUT = [Buf(f"outt{i}", bar) for i in range(6)]
            toks = []
            yi = 0
            pj = 0
            norm_stats(0, 0)
            for c in range(NCHUNK):
                ri = c % 2
                c0 = c * N
                if c + 1 < NCHUNK:
                    norm_stats(c + 1, (c + 1) % 2)
                subs = ((0, 128), (128, 128), (256, N - 256))
                for k in range(KC):
                    q = yi % 3
                    yi += 1
                    S.op("dve", lambda e, q=q, k=k, c0=c0, ri=ri: e.scalar_tensor_tensor(
                        out=Y[q][:], in0=H[:, k, c0:c0 + N], scalar=G[:, G_FIN, k:k + 1], in1=RS[ri][:],
                        op0=ALU.mult, op1=ALU.mult), reads=[bH[c], bRS[ri], bC], writes=[bY[q]])
                    for si0, (o, nt) in enumerate(subs):
                        si = (c % 2) * 3 + si0
                        pq = pj % 4
                        pj += 1
                        S.op("pe", lambda e, q=q, o=o, nt=nt, pq=pq: e.transpose(out=psum[pq][0:nt, 0:128],
                                                                                 in_=Y[q][:, o:o + nt], identity=ident_f[:]),
                             reads=[bY[q], bC], writes=[PB[pq]])
                        if (k + si0) % 2 == 0:
                            S.op("act", lambda e, si=si, k=k, nt=nt, pq=pq: e.copy(out=OUTT[si][0:nt, k * 128:(k + 1) * 128],
                                                                                   in_=psum[pq][0:nt, 0:128]),
                                 reads=[PB[pq]], writes=[bOUT[si]])
                        else:
                            S.op("dve", lambda e, si=si, k=k, nt=nt, pq=pq: e.tensor_copy(
                                out=OUTT[si][0:nt, k * 128:(k + 1) * 128], in_=psum[pq][0:nt, 0:128]),
                                reads=[PB[pq]], writes=[bOUT[si]])
                for si0, (o, nt) in enumerate(subs):
                    si = (c % 2) * 3 + si0
                    t0 = c0 + o
                    lo = max(t0, NMETA)
                    if lo >= t0 + nt:
                        continue
                    p0 = lo - t0
                    toks.append(S.dma("sp", lambda e, si=si, p0=p0, nt=nt, lo=lo, t0=t0: e.dma_start(
                        out=out[s, lo - NMETA:t0 + nt - NMETA, :], in_=OUTT[si][p0:nt, :]), out_sem[si], reads=[bOUT[si]]))
            return toks

        consts()
        out_toks = []
        ulists = []
        for s in range(nseq):
            ulists.append((conv_units(), moe_units(0), attn_units(), moe_units(1)))
        ensure_cast(4)
        for s in range(nseq):
            if s > 0:
                S.epoch()
            cu, m0, au, m1 = ulists[s]
            phase_load(s)
            done = stop_after == "load"
            if not done:
                phase_conv(cu)
                done = stop_after == "conv"
            if not done:
                phase_moe(0, m0)
                done = stop_after == "moe0"
            if not done:
                phase_attn(au)
                done = stop_after == "attn"
            if not done:
                phase_moe(1, m1)
                done = stop_after == "moe1"
            if dbg and s == 0:
                out_toks.append(dump_dbg())
            if done:
                break
            out_toks += phase_final(s)
        S.wait_only("sp", out_toks)
        S.replay()
    return nc


_NC_CACHE = {}


def kernel(**inputs):
    n_cores = 8
    if "nc" not in _NC_CACHE:
        _NC_CACHE["nc"] = build_nc()
    nc = _NC_CACHE["nc"]
    x = np.ascontiguousarray(inputs["x"], dtype=np.float32)
    shared = {}
    for name, shape in INPUT_SPECS:
        if name == "x":
            continue
        shared[name] = np.ascontiguousarray(np.asarray(inputs[name], dtype=np.float32).reshape(shape))
    in_maps = []
    for c in range(n_cores):
        m = dict(shared)
        m["x"] = np.ascontiguousarray(x[c * NSEQ:(c + 1) * NSEQ])
        in_maps.append(m)
    res = run_bass_kernel_spmd(nc, in_maps, core_ids=list(range(n_cores)))
    outs = [np.asarray(r["out"]) for r in res.results]
    return np.concatenate(outs, axis=0).astype(np.float32)
```
